# Optimizing a Trainium2 kernel written in Bass

```python
import math
import jax, jax.numpy as jnp
from jax import lax
import numpy as np

D_MODEL = 1024
BATCH = 8
SEQ = 2048
DEPTH = 2

BLOCK = 128
RET_HEADS = 4
RET_DK = 64
RET_DV = 128
RET_QK = RET_HEADS * RET_DK
RET_WIDTH = RET_HEADS * RET_DV
FOX_HEADS = 8
FOX_DH = 64
FOX_WIDTH = FOX_HEADS * FOX_DH
GM_GROUPS = 4
GM_DG = 128
GM_WIDTH = GM_GROUPS * GM_DG
BRANCH_WIDTH = 512
N_BRANCH = 3
COL_SIZES = (RET_QK, RET_QK, RET_WIDTH, RET_WIDTH,
             FOX_WIDTH, FOX_WIDTH, FOX_WIDTH, FOX_HEADS,
             GM_WIDTH, GM_WIDTH, N_BRANCH * D_MODEL)
N_IN = sum(COL_SIZES)
D_FF_DENSE = 2816
N_EXPERTS = 8
TOP_K = 2
D_FF_EXPERT = 3584
N_DENSE = (DEPTH + 1) // 2
N_MOE = DEPTH // 2
ROPE_BASE = 10000.0
LN_EPS = 1e-5
GN_EPS = 1e-6
ALPHA = (2 * DEPTH) ** 0.25
BETA = (8 * DEPTH) ** -0.25

kernel_name = "hybrid_retention_fox_gmlp_moe_deepnorm"


def layer_norm(x, g, b, eps=LN_EPS):
    xf = x.astype(jnp.float32)
    mu = xf.mean(-1, keepdims=True)
    var = jnp.square(xf - mu).mean(-1, keepdims=True)
    return ((xf - mu) * lax.rsqrt(var + eps)).astype(x.dtype) * g + b


def head_group_norm(y):
    yf = y.astype(jnp.float32)
    mu = yf.mean(-1, keepdims=True)
    var = jnp.square(yf - mu).mean(-1, keepdims=True)
    return ((yf - mu) * lax.rsqrt(var + GN_EPS)).astype(y.dtype)


def rotary(x, pos):
    half = x.shape[-1] // 2
    inv_freq = ROPE_BASE ** (-jnp.arange(half, dtype=jnp.float32) / half)
    ang = pos.astype(jnp.float32)[:, None] * inv_freq[None, :]
    cos = jnp.cos(ang)[None, :, None, :].astype(x.dtype)
    sin = jnp.sin(ang)[None, :, None, :].astype(x.dtype)
    x1, x2 = x[..., :half], x[..., half:]
    return jnp.concatenate([x1 * cos - x2 * sin, x1 * sin + x2 * cos], axis=-1)


def retention_chunkwise(q, k, v):
    bsz, s, h, dk = q.shape
    dv = v.shape[-1]
    n = s // BLOCK
    dt = q.dtype
    log_gamma = jnp.log1p(-jnp.exp2(-5.0 - jnp.arange(h, dtype=jnp.float32)))
    idx = jnp.arange(BLOCK, dtype=jnp.float32)
    rel = idx[:, None] - idx[None, :]
    causal = rel >= 0
    decay_in = jnp.where(causal[None], jnp.exp(log_gamma[:, None, None] * jnp.where(causal, rel, 0.0)[None]), 0.0).astype(dt)
    q_dec = jnp.exp(log_gamma[:, None] * (idx + 1.0)).astype(dt)
    k_dec = jnp.exp(log_gamma[:, None] * (BLOCK - 1.0 - idx)).astype(dt)
    chunk_dec = jnp.exp(log_gamma * BLOCK).astype(dt)

    def to_chunks(t):
        return t.reshape(bsz, n, BLOCK, h, t.shape[-1]).transpose(1, 0, 3, 2, 4)

    qc, kc, vc = to_chunks(q), to_chunks(k), to_chunks(v)

    def step(state, inp):
        qi, ki, vi = inp
        scores = jnp.einsum('bhid,bhjd->bhij', qi, ki) * decay_in
        inner = jnp.einsum('bhij,bhjv->bhiv', scores, vi)
        cross = jnp.einsum('bhid,bhdv->bhiv', qi * q_dec[:, :, None], state)
        new_state = state * chunk_dec[:, None, None] + jnp.einsum('bhjd,bhjv->bhdv', ki * k_dec[:, :, None], vi)
        return new_state, inner + cross

    state0 = jnp.zeros((bsz, h, dk, dv), dt)
    _, out = lax.scan(step, state0, (qc, kc, vc))
    return out.transpose(1, 0, 3, 2, 4).reshape(bsz, s, h, dv)


def forgetting_attention(q, k, v, log_f):
    s = q.shape[1]
    scale = q.shape[-1] ** -0.5
    c = jnp.cumsum(log_f, axis=1).transpose(0, 2, 1)
    outs = []
    for i in range(s // BLOCK):
        lo, hi = i * BLOCK, (i + 1) * BLOCK
        qb = q[:, lo:hi]
        kb, vb = k[:, :hi], v[:, :hi]
        logits = jnp.einsum('bqhd,bkhd->bhqk', qb, kb).astype(jnp.float32) * scale
        bias = c[:, :, lo:hi, None] - c[:, :, None, :hi]
        mask = jnp.arange(lo, hi)[:, None] >= jnp.arange(hi)[None, :]
        logits = jnp.where(mask, logits + bias, -jnp.inf)
        p = jax.nn.softmax(logits, axis=-1).astype(v.dtype)
        outs.append(jnp.einsum('bhqk,bkhd->bqhd', p, vb))
    return jnp.concatenate(outs, axis=1)


def chunked_spatial_gating(u, v, w_s, b_s):
    bsz, s, _ = u.shape
    n = s // BLOCK
    causal = jnp.tril(jnp.ones((BLOCK, BLOCK), dtype=bool))
    w = jnp.where(causal[None], w_s, 0.0)
    vc = v.reshape(bsz, n, BLOCK, GM_GROUPS, GM_DG)
    sp = jnp.einsum('gij,bnjgc->bnigc', w, vc) + b_s.T[None, None, :, :, None]
    return u * sp.reshape(bsz, s, GM_WIDTH)


def split_columns(proj):
    outs, start = [], 0
    for size in COL_SIZES:
        outs.append(proj[..., start:start + size])
        start += size
    return outs


def hybrid_mixer(x, pos, w_in, fox_b_f, gate_b, gm_w_s, gm_b_s, gm_ln_g, gm_ln_b, w_branch, w_out):
    bsz, s, _ = x.shape
    proj = x @ w_in
    (rq, rk, rv, rg, fq, fk, fv, fl, gu, gv, gl) = split_columns(proj)
    q = rotary(rq.reshape(bsz, s, RET_HEADS, RET_DK), pos)
    k = rotary(rk.reshape(bsz, s, RET_HEADS, RET_DK), pos) * (RET_DK ** -0.5)
    ret = retention_chunkwise(q, k, rv.reshape(bsz, s, RET_HEADS, RET_DV))
    y_a = jax.nn.silu(rg) * head_group_norm(ret).reshape(bsz, s, RET_WIDTH)
    log_f = jax.nn.log_sigmoid(fl.astype(jnp.float32) + fox_b_f.astype(jnp.float32))
    y_b = forgetting_attention(fq.reshape(bsz, s, FOX_HEADS, FOX_DH),
                               fk.reshape(bsz, s, FOX_HEADS, FOX_DH),
                               fv.reshape(bsz, s, FOX_HEADS, FOX_DH), log_f).reshape(bsz, s, FOX_WIDTH)
    u = jax.nn.gelu(gu)
    v = layer_norm(jax.nn.gelu(gv), gm_ln_g, gm_ln_b)
    y_c = chunked_spatial_gating(u, v, gm_w_s, gm_b_s)
    gates = jax.nn.sigmoid(gl + gate_b).reshape(bsz, s, N_BRANCH, D_MODEL)
    yb = jnp.stack([y_a, y_b, y_c], axis=2)
    branch = jnp.einsum('bsnw,nwd->bsnd', yb, w_branch)
    merged = jnp.einsum('bsnd,bsnd->bsd', gates, branch)
    return merged @ w_out


def swiglu(x, w_up, w_down):
    a, b = jnp.split(x @ w_up, 2, axis=-1)
    return (jax.nn.silu(a) * b) @ w_down


def moe_swiglu(x, w_router, w_up, w_down):
    bsz, s, d = x.shape
    t = x.reshape(-1, d)
    logits = (t @ w_router).astype(jnp.float32)
    top_val, top_idx = lax.top_k(logits, TOP_K)
    top_w = jax.nn.softmax(top_val, axis=-1)
    gate = jnp.sum(jax.nn.one_hot(top_idx, N_EXPERTS, dtype=jnp.float32) * top_w[..., None], axis=1).astype(x.dtype)
    out = jnp.zeros_like(t)
    for e in range(N_EXPERTS):
        out = out + gate[:, e:e + 1] * swiglu(t, w_up[e], w_down[e])
    return out.reshape(bsz, s, d)


def setup_inputs(seed: int = 0) -> dict:
    key = jax.random.key(seed)
    ks = jax.random.split(key, 20)
    f32 = jnp.float32
    nrm = lambda k, shape, sc: jax.random.normal(k, shape, f32) * sc
    return {
        "x": nrm(ks[0], (BATCH, SEQ, D_MODEL), 1.0),
        "w_in": nrm(ks[1], (DEPTH, D_MODEL, N_IN), D_MODEL ** -0.5),
        "fox_b_f": jax.random.uniform(ks[2], (DEPTH, FOX_HEADS), f32, 1.0, 4.0),
        "gate_b": nrm(ks[3], (DEPTH, N_BRANCH * D_MODEL), 0.02),
        "gm_w_s": nrm(ks[4], (DEPTH, GM_GROUPS, BLOCK, BLOCK), BLOCK ** -0.5),
        "gm_b_s": 1.0 + nrm(ks[5], (DEPTH, GM_GROUPS, BLOCK), 0.02),
        "gm_ln_g": 1.0 + nrm(ks[6], (DEPTH, GM_WIDTH), 0.02),
        "gm_ln_b": nrm(ks[7], (DEPTH, GM_WIDTH), 0.02),
        "w_branch": nrm(ks[8], (DEPTH, N_BRANCH, BRANCH_WIDTH, D_MODEL), BRANCH_WIDTH ** -0.5),
        "w_out": nrm(ks[9], (DEPTH, D_MODEL, D_MODEL), BETA * D_MODEL ** -0.5),
        "ln_g": 1.0 + nrm(ks[10], (DEPTH, 2, D_MODEL), 0.02),
        "ln_b": nrm(ks[11], (DEPTH, 2, D_MODEL), 0.02),
        "dense_w_up": nrm(ks[12], (N_DENSE, D_MODEL, 2 * D_FF_DENSE), D_MODEL ** -0.5),
        "dense_w_down": nrm(ks[13], (N_DENSE, D_FF_DENSE, D_MODEL), BETA * D_FF_DENSE ** -0.5),
        "moe_router": nrm(ks[14], (N_MOE, D_MODEL, N_EXPERTS), D_MODEL ** -0.5),
        "moe_w_up": nrm(ks[15], (N_MOE, N_EXPERTS, D_MODEL, 2 * D_FF_EXPERT), D_MODEL ** -0.5),
        "moe_w_down": nrm(ks[16], (N_MOE, N_EXPERTS, D_FF_EXPERT, D_MODEL), BETA * D_FF_EXPERT ** -0.5),
    }


def reference(x, w_in, fox_b_f, gate_b, gm_w_s, gm_b_s, gm_ln_g, gm_ln_b, w_branch, w_out,
              ln_g, ln_b, dense_w_up, dense_w_down, moe_router, moe_w_up, moe_w_down):
    pos = jnp.arange(x.shape[1], dtype=jnp.int32)
    for l in range(DEPTH):
        h = hybrid_mixer(x, pos, w_in[l], fox_b_f[l], gate_b[l], gm_w_s[l], gm_b_s[l],
                         gm_ln_g[l], gm_ln_b[l], w_branch[l], w_out[l])
        x = layer_norm(ALPHA * x + h, ln_g[l, 0], ln_b[l, 0])
        if l % 2 == 0:
            f = swiglu(x, dense_w_up[l // 2], dense_w_down[l // 2])
        else:
            f = moe_swiglu(x, moe_router[l // 2], moe_w_up[l // 2], moe_w_down[l // 2])
        x = layer_norm(ALPHA * x + f, ln_g[l, 1], ln_b[l, 1])
    return x
```

```python
import numpy as np
import concourse.bass as bass
import concourse.mybir as mybir
from concourse.bass_utils import run_bass_kernel_spmd

F32 = mybir.dt.float32
BF16 = mybir.dt.bfloat16
U8 = mybir.dt.uint8
AF = mybir.ActivationFunctionType
ALU = mybir.AluOpType

S = 2048
D = 1024
NT = 16
KC = 8
N_IN = 7176
DEPTH = 2
FF_DENSE = 2816
FF_EXP = 3584
NEXP = 8
ALPHA = (2 * DEPTH) ** 0.25
LN_EPS = 1e-5
GN_EPS = 1e-6
NEG = -30000.0

CF_COS, CF_SIN, CF_DECIN, CF_KDM, CF_QDEC, CF_SMALL, CF_TRI, CF_IDENT, CF_NEGM, CF_ONES = (
    0, 512, 1024, 1536, 2048, 2304, 2308, 2436, 2564, 2692)
NCF = 2820
CB_HMASK, CB_MASK, CB_IDENT = 0, 1024, 1152
NCB = 1280


def host_consts():
    lg = np.log1p(-np.exp2(-5.0 - np.arange(4, dtype=np.float64)))
    p = np.arange(128)
    cf = np.zeros((128, NCF), np.float32)
    pos = np.arange(S, dtype=np.float32)
    inv_freq = (10000.0 ** (-np.arange(32, dtype=np.float32) / 32)).astype(np.float32)
    ang = (pos[:, None] * inv_freq[None, :]).astype(np.float32)
    cos = np.cos(ang).astype(np.float32).reshape(NT, 128, 32).transpose(1, 0, 2).reshape(128, 512)
    sin = np.sin(ang).astype(np.float32).reshape(NT, 128, 32).transpose(1, 0, 2).reshape(128, 512)
    cf[:, CF_COS:CF_COS + 512] = cos
    cf[:, CF_SIN:CF_SIN + 512] = sin
    j = p[:, None]
    i = p[None, :]
    decin = np.zeros((128, 4, 128))
    kdm = np.zeros((128, 4, 128))
    qdec = np.zeros((128, 4, 64))
    for h in range(4):
        decin[:, h, :] = np.where(i >= j, np.exp(lg[h] * np.maximum(i - j, 0)), 0.0) * 0.125
        hl = h % 2
        kdm[:, h, hl * 64:(hl + 1) * 64] = (np.exp(lg[h] * (127 - p)) * 0.125)[:, None]
        qdec[:, h, :] = np.exp(lg[h] * (p + 1.0))[:, None]
    cf[:, CF_DECIN:CF_DECIN + 512] = decin.reshape(128, 512)
    cf[:, CF_KDM:CF_KDM + 512] = kdm.reshape(128, 512)
    cf[:, CF_QDEC:CF_QDEC + 256] = qdec.reshape(128, 256)
    cf[:, CF_SMALL + 0] = (p < 64)
    cf[:, CF_SMALL + 1] = (p >= 64)
    for pr in range(2):
        cf[:, CF_SMALL + 2 + pr] = np.exp(np.where(p < 64, lg[2 * pr], lg[2 * pr + 1]) * 128.0)
    tri = (j <= i).astype(np.float32)
    cf[:, CF_TRI:CF_TRI + 128] = tri
    cf[:, CF_IDENT:CF_IDENT + 128] = np.eye(128, dtype=np.float32)
    cf[:, CF_NEGM:CF_NEGM + 128] = np.where(j <= i, 0.0, NEG)
    cf[:, CF_ONES:CF_ONES + 128] = 1.0
    cb = np.zeros((128, NCB), np.float32)
    hm = np.zeros((128, 8, 128), np.float32)
    for h in range(8):
        hm[h * 6:(h + 1) * 6, h, :] = 1.0
    cb[:, CB_HMASK:CB_HMASK + 1024] = hm.reshape(128, 1024)
    cb[:, CB_MASK:CB_MASK + 128] = tri
    cb[:, CB_IDENT:CB_IDENT + 128] = np.eye(128, dtype=np.float32)
    return cf, cb


class Eng:
    def __init__(self, nc, eng, name):
        self.eng = eng
        self.sem = nc.alloc_semaphore("s_" + name)
        self.cnt = 0
        self.seen = {}
        self.name = name


class DSem:
    def __init__(self, nc, name):
        self.sem = nc.alloc_semaphore("d_" + name)
        self.cnt = 0
        self.name = name


class Res:
    __slots__ = ("w", "r")

    def __init__(self):
        self.w = None
        self.r = {}


class Cut(Exception):
    pass


class KB:
    def ck(self, n):
        if self.cutn == n:
            raise Cut()

    def __init__(self, n_layers=2, debug=False, stop_after=None, cutn=None):
        self.cutn = cutn
        self.n_layers = n_layers
        self.debug = debug
        self.stop_after = stop_after
        self.taps = []
        nc = self.nc = bass.Bass("TRN2", target_bir_lowering=False)
        self.pe = Eng(nc, nc.tensor, "pe")
        self.act = Eng(nc, nc.scalar, "act")
        self.dve = Eng(nc, nc.vector, "dve")
        self.pool = Eng(nc, nc.gpsimd, "pool")
        self.sp = Eng(nc, nc.sync, "sp")
        self.engs = [self.pe, self.act, self.dve, self.pool, self.sp]
        self.dsems = {}
        self.res = {}
        self.inputs = {}
        di = self.din
        self.x = di("x", [S, D])
        self.constf = di("constf", [128, NCF])
        self.constb = di("constb", [128, NCB])
        L = n_layers
        self.w_in = [di(f"w_in{l}", [D, N_IN]) for l in range(L)]
        self.fox_b = [di(f"fox_b{l}", [1, 8]) for l in range(L)]
        self.gate_bT = [di(f"gate_bT{l}", [128, 24]) for l in range(L)]
        self.gm_wsT = [di(f"gm_wsT{l}", [128, 512]) for l in range(L)]
        self.gm_bsT = [di(f"gm_bsT{l}", [128, 4]) for l in range(L)]
        self.gm_ln = [di(f"gm_ln{l}", [2, 512]) for l in range(L)]
        self.w_branch = [di(f"w_branch{l}", [3, 512, D]) for l in range(L)]
        self.w_out = [di(f"w_out{l}", [D, D]) for l in range(L)]
        self.ln_gb = [di(f"ln_gb{l}", [4, D]) for l in range(L)]
        self.dense_up = di("dense_up", [D, 2 * FF_DENSE])
        self.dense_down = di("dense_down", [FF_DENSE, D])
        if L > 1:
            self.router = di("router", [D, NEXP])
            self.moe_up = di("moe_up", [NEXP, D, 2 * FF_EXP])
            self.moe_down = di("moe_down", [NEXP, FF_EXP, D])
        self.y = nc.dram_tensor("y", [S, D], F32, kind="ExternalOutput").ap()
        self.xres = nc.dram_tensor("xres", [S, D], F32).ap()
        nbytes = (int(nc.sbuf_bytes_remaining) // 64) * 64 - 64
        self.arena_bytes = nbytes
        self.arena = nc.alloc_sbuf_tensor("arena", [128, nbytes], U8)
        self.psum = nc.alloc_psum_tensor("psum", [128, 4096], F32)
        self.off = 0

    def din(self, name, shape):
        t = self.nc.dram_tensor(name, list(shape), F32, kind="ExternalInput").ap()
        self.inputs[name] = tuple(shape)
        return t

    def A(self, shape, dt):
        esz = 4 if dt == F32 else 2
        n = int(np.prod(shape[1:]))
        nb = n * esz
        off = self.off
        assert off + nb <= self.arena_bytes, ("SBUF arena overflow", off, nb, self.arena_bytes)
        ap = self.arena[:, off:off + nb].bitcast(dt)
        if len(shape) == 3:
            ap = ap.rearrange("p (a b) -> p a b", a=shape[1])
        elif len(shape) == 4:
            ap = ap.rearrange("p (a b c) -> p a b c", a=shape[1], b=shape[2])
        self.off = off + ((nb + 63) // 64) * 64
        return ap

    def bank(self, b, dt=F32):
        ap = self.psum[:, b * 512:(b + 1) * 512]
        if dt == BF16:
            ap = ap.bitcast(BF16)
        return ap

    def R(self, k):
        r = self.res.get(k)
        if r is None:
            r = self.res[k] = Res()
        return r

    def dsem(self, name):
        s = self.dsems.get(name)
        if s is None:
            s = self.dsems[name] = DSem(self.nc, name)
        return s

    def wait(self, e, so, v):
        if v <= 0 or e.seen.get(so, 0) >= v:
            return
        e.eng.wait_ge(so.sem, v)
        e.seen[so] = v

    def _deps(self, e, reads, writes):
        for k in reads:
            R = self.R(k)
            if R.w is not None:
                so, v = R.w
                if not (so is e and e is self.pe):
                    self.wait(e, so, v)
        for k in writes:
            R = self.R(k)
            if R.w is not None:
                so, v = R.w
                if so is not e:
                    self.wait(e, so, v)
            for so, v in R.r.items():
                if so is not e:
                    self.wait(e, so, v)

    def op(self, e, fn, reads=(), writes=(), inc=True):
        if e is not self.pe:
            extra = [k for k in reads if isinstance(k, str) and k[0] == "B" and k[1:].isdigit() and k not in writes]
            if extra:
                writes = list(writes) + extra
        self._deps(e, reads, writes)
        ins = fn()
        if inc:
            ins.then_inc(e.sem, 1)
            e.cnt += 1
            tag = e.cnt
        else:
            tag = e.cnt + 1
        for k in reads:
            R = self.R(k)
            R.r[e] = max(R.r.get(e, 0), tag)
        for k in writes:
            R = self.R(k)
            R.w = (e, tag)
            R.r = {}
        return ins

    def dma(self, q, out, in_, reads=(), writes=(), sem="misc", **kw):
        Sm = self.dsem(sem)
        self._deps(q, reads, writes)
        q.eng.dma_start(out=out, in_=in_, **kw).then_inc(Sm.sem, 16)
        Sm.cnt += 16
        for k in reads:
            self.R(k).r[Sm] = Sm.cnt
        for k in writes:
            R = self.R(k)
            R.w = (Sm, Sm.cnt)
            R.r = {}

    def barrier(self):
        for e in self.engs:
            for o in self.engs:
                if o is not e:
                    self.wait(e, o, o.cnt)
            for s in self.dsems.values():
                self.wait(e, s, s.cnt)

    def tap(self, name, ap, shape, reads=()):
        if not self.debug:
            return
        t = self.nc.dram_tensor("dbg_" + name, list(shape), ap.dtype, kind="ExternalOutput").ap()
        self.dma(self.sp, out=t, in_=ap, reads=reads, sem="dbg")
        self.taps.append("dbg_" + name)

    def mm(self, out, lhsT, rhs, start, stop, reads=(), writes=(), inc=None):
        if inc is None:
            inc = stop
        return self.op(self.pe, lambda: self.nc.tensor.matmul(out, lhsT=lhsT, rhs=rhs, start=start, stop=stop),
                       reads, writes, inc)

    def tr(self, out, in_, ident, reads=(), writes=(), inc=True):
        return self.op(self.pe, lambda: self.nc.tensor.transpose(out=out, in_=in_, identity=ident),
                       reads, writes, inc)

    def actf(self, out, in_, func, reads=(), writes=(), **kw):
        return self.op(self.act, lambda: self.nc.scalar.activation(out=out, in_=in_, func=func, **kw), reads, writes)

    def tt(self, out, in0, in1, op, reads=(), writes=()):
        return self.op(self.dve, lambda: self.nc.vector.tensor_tensor(out=out, in0=in0, in1=in1, op=op), reads, writes)

    def ts(self, out, in0, s1, s2, op0, op1=None, reads=(), writes=()):
        if op1 is None:
            return self.op(self.dve, lambda: self.nc.vector.tensor_scalar(out=out, in0=in0, scalar1=s1, scalar2=None, op0=op0),
                           reads, writes)
        return self.op(self.dve, lambda: self.nc.vector.tensor_scalar(out=out, in0=in0, scalar1=s1, scalar2=s2, op0=op0, op1=op1),
                       reads, writes)

    def stt(self, out, in0, scalar, in1, op0, op1, reads=(), writes=()):
        return self.op(self.dve, lambda: self.nc.vector.scalar_tensor_tensor(out=out, in0=in0, scalar=scalar, in1=in1, op0=op0, op1=op1),
                       reads, writes)

    def vcopy(self, out, in_, reads=(), writes=()):
        return self.op(self.dve, lambda: self.nc.vector.tensor_copy(out=out, in_=in_), reads, writes)

    def ln_stats(self, src, n, eps, key_src, tmp):
        st, mv, rstd = tmp
        nch = (n + 511) // 512
        for c in range(nch):
            w = min(512, n - c * 512)
            self.op(self.dve, lambda c=c, w=w: self.nc.vector.bn_stats(out=st[:, c, :], in_=src[:, c * 512:c * 512 + w]),
                    reads=[key_src], writes=["ln_st"])
        self.op(self.dve, lambda: self.nc.vector.bn_aggr(out=mv, in_=st[:, 0:nch, :].rearrange("p a b -> p (a b)")),
                reads=["ln_st"], writes=["ln_mv"])
        self.actf(rstd, mv[:, 1:2], AF.Sqrt, reads=["ln_mv"], writes=["ln_rstd"], bias=eps, scale=1.0)
        self.op(self.dve, lambda: self.nc.vector.reciprocal(out=rstd, in_=rstd), reads=["ln_rstd"], writes=["ln_rstd"])
        return mv, rstd

    def build(self):
        nc = self.nc
        pe, act, dve, pool, sp = self.pe, self.act, self.dve, self.pool, self.sp
        self.xT = self.A([128, KC, S], BF16)
        self.cf = self.A([128, NCF], F32)
        self.cb = self.A([128, NCB], BF16)
        cf, cb = self.cf, self.cb
        self.cosv = cf[:, CF_COS:CF_COS + 512].rearrange("p (a b) -> p a b", a=NT)
        self.sinv = cf[:, CF_SIN:CF_SIN + 512].rearrange("p (a b) -> p a b", a=NT)
        self.decin = cf[:, CF_DECIN:CF_DECIN + 512]
        self.kdm = cf[:, CF_KDM:CF_KDM + 512].rearrange("p (a b) -> p a b", a=4)
        self.qdec = cf[:, CF_QDEC:CF_QDEC + 256]
        self.pmask = cf[:, CF_SMALL:CF_SMALL + 2]
        self.sdec = cf[:, CF_SMALL + 2:CF_SMALL + 4]
        self.tri = cf[:, CF_TRI:CF_TRI + 128]
        self.identf = cf[:, CF_IDENT:CF_IDENT + 128]
        self.negm = cf[:, CF_NEGM:CF_NEGM + 128]
        self.onesf = cf[:, CF_ONES:CF_ONES + 128]
        self.hmask = cb[:, CB_HMASK:CB_HMASK + 1024].rearrange("p (a b) -> p a b", a=8)
        self.mask01 = cb[:, CB_MASK:CB_MASK + 128]
        self.identb = cb[:, CB_IDENT:CB_IDENT + 128]
        self.lng = self.A([128, D], F32)
        self.lnb = self.A([128, D], F32)
        self.gates = self.A([128, NT, NEXP], F32)
        self.st = self.A([128, 4, 6], F32)
        self.mv = self.A([128, 4, 2], F32)
        self.rstd = self.A([128, 4], F32)
        self.lnst = self.A([128, 2, 6], F32)
        self.lnmv = self.A([128, 2], F32)
        self.lnrstd = self.A([128, 1], F32)
        self.MBASE = self.off
        self.dma(sp, out=cf, in_=self.constf[:, :], writes=["cf"], sem="const")
        self.dma(pool, out=cb, in_=self.constb[:, :], writes=["cb"], sem="const")

        self.off = self.MBASE
        xbs = [self.A([128, D], BF16) for _ in range(2)]
        for i in range(NT):
            b = i % 2
            self.dma(pool, out=xbs[b], in_=self.x[i * 128:(i + 1) * 128, :], writes=[f"xb{b}"], sem=f"xb{b}")
            psT = self.bank(b, BF16).rearrange("p (a b) -> p a b", a=8)
            for kc in range(KC):
                self.tr(psT[:, kc, :], xbs[b][:, kc * 128:(kc + 1) * 128], self.identb,
                        reads=[f"xb{b}", "cb"], writes=[f"ps{b}"], inc=(kc == KC - 1))
            self.actf(self.xT[:, :, i * 128:(i + 1) * 128], psT, AF.Copy, reads=[f"ps{b}"], writes=[("xT", i)])
        self.barrier()
        if self.stop_after == ("P0", 0):
            self.tap("xT", self.xT, [128, KC, S])
            self.barrier()
            return nc

        for l in range(self.n_layers):
            try:
                self.mixer(l)
            except Cut:
                self.barrier()
                return nc
            if self.stop_after is not None and self.stop_after[0] in ("P1", "P2", "P3", "P4", "P5") and self.stop_after[1] == l:
                break
            if self.stop_after == ("mixer", l):
                break
            self.ffn(l)
            if self.stop_after == ("ffn", l):
                break
        self.barrier()
        return nc

    def mixer(self, l):
        nc = self.nc
        pe, act, dve, pool, sp = self.pe, self.act, self.dve, self.pool, self.sp
        xT = self.xT
        w_in = self.w_in[l].rearrange("(kc p) n -> p kc n", p=128)
        self.off = self.MBASE
        YT = self.A([128, 12, S], BF16)
        P_OFF = self.off
        B = [self.bank(b) for b in range(8)]
        Bb = [self.bank(b, BF16) for b in range(8)]

        wA = self.A([128, KC, 1536], BF16)
        self.dma(pool, out=wA, in_=w_in[:, :, 0:1536], writes=["wA"], sem="wA")
        rot = self.A([128, 8, 2, 32], F32)
        t1 = self.A([128, 8, 32], F32)
        t2 = self.A([128, 8, 32], F32)
        rot_bf = self.A([128, 512], BF16)
        qp_bf = self.A([128, 256], BF16)
        kpm = self.A([128, 4, 128], BF16)
        qTm = self.A([128, 4, 128], BF16)
        qpTm = self.A([128, 4, 128], BF16)
        kT = self.A([128, 2, 128], BF16)
        ST = self.A([128, 4, 128], BF16)
        v_bf = self.A([128, 512], BF16)
        Sst = self.A([128, 2, 128], F32)
        Sbf = self.A([128, 2, 128], BF16)
        yn = self.A([128, 512], F32)
        sg = self.A([128, 512], F32)
        ya = self.A([128, 512], BF16)
        self.op(dve, lambda: nc.vector.memset(Sst, 0.0), writes=["Sst"])
        self.op(dve, lambda: nc.vector.memset(Sbf, 0.0), writes=["Sbf"])
        for i in range(NT):
            tsl = slice(i * 128, (i + 1) * 128)
            for c in range(3):
                for kc in range(KC):
                    self.mm(B[c], xT[:, kc, tsl], wA[:, kc, c * 512:(c + 1) * 512], kc == 0, kc == KC - 1,
                            reads=["wA"], writes=[f"B{c}"])
            self.ck(1)
            qk = B[0].rearrange("p (g h d) -> p g h d", g=8, h=2)
            x1, x2 = qk[:, :, 0, :], qk[:, :, 1, :]
            cb_ = self.cosv[:, i, :].unsqueeze(1).broadcast_to([128, 8, 32])
            sb_ = self.sinv[:, i, :].unsqueeze(1).broadcast_to([128, 8, 32])
            self.tt(t1, x1, cb_, ALU.mult, reads=["B0", "cf"], writes=["t1"])
            self.tt(t2, x2, sb_, ALU.mult, reads=["B0"], writes=["t2"])
            self.tt(rot[:, :, 0, :], t1, t2, ALU.subtract, reads=["t1", "t2"], writes=["rot0"])
            self.tt(t1, x1, sb_, ALU.mult, reads=["B0"], writes=["t1"])
            self.tt(t2, x2, cb_, ALU.mult, reads=["B0"], writes=["t2"])
            self.tt(rot[:, :, 1, :], t1, t2, ALU.add, reads=["t1", "t2"], writes=["rot1"])
            self.ck(2)
            rotf = rot.rearrange("p g h d -> p (g h d)")
            self.actf(rot_bf, rotf, AF.Copy, reads=["rot0", "rot1"], writes=["rot_bf"])
            self.tt(qp_bf, rotf[:, 0:256], self.qdec, ALU.mult, reads=["rot0", "rot1"], writes=["qp_bf"])
            for h in range(4):
                pr = h // 2
                self.tt(kpm[:, h, :], rotf[:, 256 + pr * 128:256 + (pr + 1) * 128], self.kdm[:, h, :], ALU.mult,
                        reads=["rot0", "rot1"], writes=["kpm"])
            self.actf(v_bf, B[1], AF.Copy, reads=["B1"], writes=["v_bf"])
            self.ck(3)
            psT = Bb[3].rearrange("p (a b) -> p a b", a=8)
            srcs = [rot_bf[:, 0:128], rot_bf[:, 128:256], rot_bf[:, 256:384], rot_bf[:, 384:512],
                    qp_bf[:, 0:128], qp_bf[:, 128:256]]
            for k_, s_ in enumerate(srcs):
                self.tr(psT[:, k_, :], s_, self.identb, reads=["rot_bf", "qp_bf", "cb"], writes=["B3"], inc=(k_ == 5))
            self.ck(31)
            qTm4 = qTm.rearrange("p (a b) t -> p a b t", a=2)
            qpTm4 = qpTm.rearrange("p (a b) t -> p a b t", a=2)
            for par in range(2):
                self.ts(qTm4[:, :, par, :], psT[:, 0:2, :], self.pmask[:, par:par + 1], None, ALU.mult,
                        reads=["B3"], writes=["qTm"])
                self.ts(qpTm4[:, :, par, :], psT[:, 4:6, :], self.pmask[:, par:par + 1], None, ALU.mult,
                        reads=["B3"], writes=["qpTm"])
            self.ck(32)
            self.actf(kT, psT[:, 2:4, :], AF.Copy, reads=["B3"], writes=["kT"])
            self.ck(4)
            for h in range(4):
                self.mm(B[4][:, h * 128:(h + 1) * 128], kT[:, h // 2, :], qTm[:, h, :], True, True,
                        reads=["kT", "qTm"], writes=["B4"], inc=(h == 3))
            self.tt(ST.rearrange("p a b -> p (a b)"), B[4], self.decin, ALU.mult, reads=["B4"], writes=["ST"])
            self.ck(5)
            for h in range(4):
                self.mm(B[5][:, h * 128:(h + 1) * 128], ST[:, h, :], v_bf[:, h * 128:(h + 1) * 128], True, False,
                        reads=["ST", "v_bf"], writes=["B5"], inc=False)
                self.mm(B[5][:, h * 128:(h + 1) * 128], qpTm[:, h, :], Sbf[:, h // 2, :], False, True,
                        reads=["qpTm", "Sbf"], writes=["B5"], inc=(h == 3))
            for pr in range(2):
                for hl in range(2):
                    h = 2 * pr + hl
                    self.mm(B[6][:, pr * 128:(pr + 1) * 128], kpm[:, h, :], v_bf[:, h * 128:(h + 1) * 128],
                            hl == 0, hl == 1, reads=["kpm", "v_bf"], writes=["B6"], inc=(pr == 1 and hl == 1))
            for pr in range(2):
                self.stt(Sst[:, pr, :], Sst[:, pr, :], self.sdec[:, pr:pr + 1], B[6][:, pr * 128:(pr + 1) * 128],
                         ALU.mult, ALU.add, reads=["Sst", "B6"], writes=["Sst"])
            self.actf(Sbf, Sst, AF.Copy, reads=["Sst"], writes=["Sbf"])
            self.ck(6)
            for h in range(4):
                self.op(dve, lambda h=h: nc.vector.bn_stats(out=self.st[:, h, :], in_=B[5][:, h * 128:(h + 1) * 128]),
                        reads=["B5"], writes=["st"])
            for h in range(4):
                self.op(dve, lambda h=h: nc.vector.bn_aggr(out=self.mv[:, h, :], in_=self.st[:, h, :]),
                        reads=["st"], writes=["mv"])
            self.actf(self.rstd, self.mv[:, :, 1], AF.Sqrt, reads=["mv"], writes=["rstd"], bias=GN_EPS, scale=1.0)
            self.op(dve, lambda: nc.vector.reciprocal(out=self.rstd, in_=self.rstd), reads=["rstd"], writes=["rstd"])
            for h in range(4):
                self.ts(yn[:, h * 128:(h + 1) * 128], B[5][:, h * 128:(h + 1) * 128], self.mv[:, h, 0:1],
                        self.rstd[:, h:h + 1], ALU.subtract, ALU.mult, reads=["B5", "mv", "rstd"], writes=["yn"])
            self.actf(sg, B[2], AF.Silu, reads=["B2"], writes=["sg"])
            self.tt(ya, yn, sg, ALU.mult, reads=["yn", "sg"], writes=["ya"])
            self.ck(7)
            psY = Bb[7].rearrange("p (a b) -> p a b", a=8)
            for k_ in range(4):
                self.tr(psY[:, k_, :], ya[:, k_ * 128:(k_ + 1) * 128], self.identb, reads=["ya"], writes=["B7"], inc=(k_ == 3))
            self.actf(YT[:, 0:4, tsl], psY[:, 0:4, :], AF.Copy, reads=["B7"], writes=[("YT", i)])
        self.barrier()
        if self.stop_after == ("P1", l):
            self.tap("YT", YT, [128, 12, S])
            return

        self.off = P_OFF
        wB = self.A([128, KC, 1544], BF16)
        self.dma(pool, out=wB, in_=w_in[:, :, 1536:3080], writes=["wB"], sem="wA")
        foxb = self.A([128, 8], F32)
        self.dma(sp, out=foxb, in_=self.fox_b[l][0:1, :].partition_broadcast(128), writes=["foxb"], sem="small")
        KT = self.A([128, 4, S], BF16)
        KBc = self.A([128, S], BF16)
        Vaug = self.A([128, NT, 8, 65], BF16)
        q_bf = self.A([128, 512], BF16)
        k_bf = self.A([128, 512], BF16)
        z = self.A([128, 8], F32)
        ez = self.A([128, 8], F32)
        lf = self.A([128, 8], F32)
        negc = self.A([128, 8], F32)
        carry = self.A([128, 8], F32)
        r1 = self.A([128, 8], F32)
        r2 = self.A([128, 8], F32)
        BK = self.A([128, 128], BF16)
        BQ = self.A([128, 128], BF16)
        qTm8 = self.A([128, 8, 128], BF16)
        QBm = self.A([128, 8, 128], BF16)
        PT = [self.A([128, NT, 128], BF16) for _ in range(2)]
        rs = self.A([128, 8], F32)
        yb = self.A([128, 8, 64], BF16)
        self.op(dve, lambda: nc.vector.memset(carry, 0.0), writes=["carry"])
        self.op(dve, lambda: nc.vector.memset(BK, 0.0), writes=["BK"])
        self.op(dve, lambda: nc.vector.memset(BQ, 0.0), writes=["BQ"])
        self.op(dve, lambda: nc.vector.memset(Vaug.rearrange("p a b c -> p (a b c)"), 1.0), writes=["Vaug"])
        BK3 = BK[:, 0:48].rearrange("p (h r) -> p h r", h=8)
        BQ3 = BQ[:, 0:48].rearrange("p (h r) -> p h r", h=8)
        self.op(dve, lambda: nc.vector.memset(BK3[:, :, 3:6], 1.0), reads=[], writes=["BK"])
        self.op(dve, lambda: nc.vector.memset(BQ3[:, :, 0:3], 1.0), reads=[], writes=["BQ"])
        chunkcnt = 0
        for i in range(NT):
            tsl = slice(i * 128, (i + 1) * 128)
            for kc in range(KC):
                self.mm(B[0], xT[:, kc, tsl], wB[:, kc, 0:512], kc == 0, kc == KC - 1, reads=["wB"], writes=["B0"])
            for kc in range(KC):
                self.mm(B[1], xT[:, kc, tsl], wB[:, kc, 512:1024], kc == 0, kc == KC - 1, reads=["wB"], writes=["B1"])
            self.actf(q_bf, B[0], AF.Copy, reads=["B0"], writes=["q_bf"])
            self.actf(k_bf, B[1], AF.Identity, reads=["B1"], writes=["k_bf"], scale=0.125)
            for kc in range(KC):
                self.mm(B[0], xT[:, kc, tsl], wB[:, kc, 1024:1536], kc == 0, kc == KC - 1, reads=["wB"], writes=["B0"])
            for kc in range(KC):
                self.mm(B[1][:, 0:8], xT[:, kc, tsl], wB[:, kc, 1536:1544], kc == 0, kc == KC - 1, reads=["wB"], writes=["B1"])
            self.actf(Vaug[:, i, :, 0:64], B[0].rearrange("p (h d) -> p h d", h=8), AF.Copy, reads=["B0"], writes=[("V", i)])
            self.tt(z, B[1][:, 0:8], foxb, ALU.add, reads=["B1", "foxb"], writes=["z"])
            self.actf(ez, z, AF.Exp, reads=["z"], writes=["ez"], scale=-1.0)
            self.actf(lf, ez, AF.Ln, reads=["ez"], writes=["lf"], bias=1.0, scale=1.0)
            self.mm(B[1][:, 8:16], self.tri, lf, True, True, reads=["lf", "cf"], writes=["B1"], inc=False)
            self.mm(B[1][:, 16:24], self.onesf, lf, True, True, reads=["lf"], writes=["B1"], inc=True)
            self.tt(negc, B[1][:, 8:16], carry, ALU.add, reads=["B1", "carry"], writes=["negc"])
            self.tt(carry, B[1][:, 16:24], carry, ALU.add, reads=["B1", "carry"], writes=["carry"])
            self.vcopy(BK3[:, :, 0], negc, reads=["negc"], writes=["BKa"])
            self.tt(r1, negc, BK3[:, :, 0], ALU.subtract, reads=["negc", "BKa"], writes=["r1"])
            self.vcopy(BK3[:, :, 1], r1, reads=["r1"], writes=["BKb"])
            self.tt(r2, r1, BK3[:, :, 1], ALU.subtract, reads=["r1", "BKb"], writes=["r2"])
            self.vcopy(BK3[:, :, 2], r2, reads=["r2"], writes=["BKc"])
            self.ts(BQ3[:, :, 3:6], BK3[:, :, 0:3], -1.0, None, ALU.mult, reads=["BKa", "BKb", "BKc"], writes=["BQ"])
            psT1 = Bb[2].rearrange("p (a b) -> p a b", a=8)
            psT2 = Bb[3].rearrange("p (a b) -> p a b", a=8)
            for k_ in range(4):
                self.tr(psT1[:, k_, :], q_bf[:, k_ * 128:(k_ + 1) * 128], self.identb, reads=["q_bf", "cb"], writes=["B2"], inc=False)
            for k_ in range(4):
                self.tr(psT1[:, 4 + k_, :], k_bf[:, k_ * 128:(k_ + 1) * 128], self.identb, reads=["k_bf"], writes=["B2"], inc=(k_ == 3))
            self.tr(psT2[:, 0, :], BK, self.identb, reads=["BK", "BKa", "BKb", "BKc"], writes=["B3"], inc=False)
            self.tr(psT2[:, 1, :], BQ, self.identb, reads=["BQ"], writes=["B3"], inc=True)
            qTm84 = qTm8.rearrange("p (a b) t -> p a b t", a=4)
            for par in range(2):
                self.ts(qTm84[:, :, par, :], psT1[:, 0:4, :], self.pmask[:, par:par + 1], None, ALU.mult,
                        reads=["B2"], writes=["qTm8"])
            self.actf(KT[:, :, tsl], psT1[:, 4:8, :], AF.Copy, reads=["B2"], writes=[("KT", i)])
            self.actf(KBc[:, tsl], psT2[:, 0, :], AF.Copy, reads=["B3"], writes=[("KB", i)])
            self.tt(QBm, psT2[:, 1, :].unsqueeze(1).broadcast_to([128, 8, 128]), self.hmask, ALU.mult,
                    reads=["B3", "cb"], writes=["QBm"])
            for h in range(8):
                PTb = PT[h % 2]
                pk = f"PT{h % 2}"
                for c0 in range(0, i + 1, 4):
                    nb = min(4, i + 1 - c0)
                    bk = 4 + (chunkcnt % 2)
                    chunkcnt += 1
                    for jj in range(nb):
                        j = c0 + jj
                        self.mm(B[bk][:, jj * 128:(jj + 1) * 128], KT[:, h // 2, j * 128:(j + 1) * 128], qTm8[:, h, :],
                                True, False, reads=[("KT", j), "qTm8"], writes=[f"B{bk}"], inc=False)
                        self.mm(B[bk][:, jj * 128:(jj + 1) * 128], KBc[:, j * 128:(j + 1) * 128], QBm[:, h, :],
                                False, True, reads=[("KB", j), "QBm"], writes=[f"B{bk}"], inc=(jj == nb - 1))
                    if c0 + nb == i + 1:
                        jj = nb - 1
                        self.tt(B[bk][:, jj * 128:(jj + 1) * 128], B[bk][:, jj * 128:(jj + 1) * 128], self.negm, ALU.add,
                                reads=[f"B{bk}"], writes=[f"B{bk}"])
                    self.actf(PTb[:, c0:c0 + nb, :].rearrange("p a b -> p (a b)"), B[bk][:, 0:nb * 128], AF.Exp,
                              reads=[f"B{bk}"], writes=[pk])
                ob = 6 + h // 4
                for j in range(i + 1):
                    self.mm(B[ob][:, (h % 4) * 65:(h % 4) * 65 + 65], PTb[:, j, :], Vaug[:, j, h, :], j == 0, j == i,
                            reads=[pk, ("V", j), "Vaug"], writes=[f"B{ob}"], inc=(j == i))
            for hb in range(2):
                po = B[6 + hb][:, 0:260].rearrange("p (h c) -> p h c", h=4)
                self.op(dve, lambda po=po, hb=hb: nc.vector.reciprocal(out=rs[:, hb * 4:(hb + 1) * 4], in_=po[:, :, 64]),
                        reads=[f"B{6 + hb}"], writes=["rs"])
                self.tt(yb[:, hb * 4:(hb + 1) * 4, :], po[:, :, 0:64],
                        rs[:, hb * 4:(hb + 1) * 4].unsqueeze(2).broadcast_to([128, 4, 64]), ALU.mult,
                        reads=[f"B{6 + hb}", "rs"], writes=["yb"])
            ybf = yb.rearrange("p a b -> p (a b)")
            for k_ in range(4):
                self.tr(psT2[:, 2 + k_, :], ybf[:, k_ * 128:(k_ + 1) * 128], self.identb, reads=["yb"], writes=["B3"], inc=(k_ == 3))
            self.actf(YT[:, 4:8, tsl], psT2[:, 2:6, :], AF.Copy, reads=["B3"], writes=[("YT", i)])
        self.barrier()
        if self.stop_after == ("P2", l):
            self.tap("YT", YT, [128, 12, S])
            return

        self.off = P_OFF
        wC = self.A([128, KC, 1024], BF16)
        self.dma(pool, out=wC, in_=w_in[:, :, 3080:4104], writes=["wC"], sem="wA")
        wsf = self.A([128, 512], F32)
        WST = self.A([128, 4, 128], BF16)
        bsT = self.A([128, 4], F32)
        gmg = self.A([128, 512], F32)
        gmb = self.A([128, 512], F32)
        self.dma(sp, out=wsf, in_=self.gm_wsT[l][:, :], writes=["wsf"], sem="small")
        self.dma(sp, out=bsT, in_=self.gm_bsT[l][:, :], writes=["bsT"], sem="small")
        self.dma(sp, out=gmg, in_=self.gm_ln[l][0:1, :].partition_broadcast(128), writes=["gmg"], sem="small")
        self.dma(sp, out=gmb, in_=self.gm_ln[l][1:2, :].partition_broadcast(128), writes=["gmb"], sem="small")
        for r_ in ("wsf", "bsT", "gmg", "gmb", "foxb"):
            self.R(r_).w = (self.dsem("small"), self.dsem("small").cnt)
        self.tt(WST, wsf.rearrange("p (a b) -> p a b", a=4), self.mask01.unsqueeze(1).broadcast_to([128, 4, 128]),
                ALU.mult, reads=["wsf", "cb"], writes=["WST"])
        u = self.A([128, 512], F32)
        gvv = self.A([128, 512], F32)
        vb = self.A([128, 512], BF16)
        yc = self.A([128, 512], BF16)
        for i in range(NT):
            tsl = slice(i * 128, (i + 1) * 128)
            for c in range(2):
                for kc in range(KC):
                    self.mm(B[c], xT[:, kc, tsl], wC[:, kc, c * 512:(c + 1) * 512], kc == 0, kc == KC - 1,
                            reads=["wC"], writes=[f"B{c}"])
            self.actf(u, B[0], AF.Gelu_apprx_tanh, reads=["B0"], writes=["u"])
            self.actf(gvv, B[1], AF.Gelu_apprx_tanh, reads=["B1"], writes=["gvv"])
            mv, rstd = self.ln_stats(gvv, 512, LN_EPS, "gvv", (self.lnst, self.lnmv, self.lnrstd))
            self.ts(gvv, gvv, mv[:, 0:1], rstd[:, 0:1], ALU.subtract, ALU.mult, reads=["gvv", "ln_mv", "ln_rstd"], writes=["gvv"])
            self.tt(gvv, gvv, gmg, ALU.mult, reads=["gvv", "gmg"], writes=["gvv"])
            self.tt(vb, gvv, gmb, ALU.add, reads=["gvv", "gmb"], writes=["vb"])
            for g in range(4):
                self.mm(B[2][:, g * 128:(g + 1) * 128], WST[:, g, :], vb[:, g * 128:(g + 1) * 128], True, True,
                        reads=["WST", "vb"], writes=["B2"], inc=(g == 3))
            for g in range(4):
                self.stt(yc[:, g * 128:(g + 1) * 128], B[2][:, g * 128:(g + 1) * 128], bsT[:, g:g + 1],
                         u[:, g * 128:(g + 1) * 128], ALU.add, ALU.mult, reads=["B2", "bsT", "u"], writes=["yc"])
            psT = Bb[3].rearrange("p (a b) -> p a b", a=8)
            for k_ in range(4):
                self.tr(psT[:, k_, :], yc[:, k_ * 128:(k_ + 1) * 128], self.identb, reads=["yc"], writes=["B3"], inc=(k_ == 3))
            self.actf(YT[:, 8:12, tsl], psT[:, 0:4, :], AF.Copy, reads=["B3"], writes=[("YT", i)])
        self.barrier()
        if self.stop_after == ("P3", l):
            self.tap("YT", YT, [128, 12, S])
            return

        self.off = self.MBASE + 65536
        mergedT = self.A([128, KC, S], BF16)
        P5_OFF = self.off
        gbT = self.A([128, 24], F32)
        self.dma(sp, out=gbT, in_=self.gate_bT[l][:, :], writes=["gbT"], sem="small")
        wG = [self.A([128, KC, 3, 128], BF16) for _ in range(2)]
        wBr = [self.A([128, 4, 3, 128], BF16) for _ in range(2)]
        sgt = [self.A([128, 512], F32) for _ in range(2)]
        macc = self.A([128, 512], F32)
        mtmp = self.A([128, 512], F32)
        w_br = self.w_branch[l]

        def load_g(dc):
            s_ = dc % 2
            for n in range(3):
                c0 = 4104 + n * 1024 + dc * 128
                self.dma(pool, out=wG[s_][:, :, n, :], in_=w_in[:, :, c0:c0 + 128], writes=[f"wG{s_}"], sem=f"wG{s_}")
                self.dma(pool, out=wBr[s_][:, :, n, :],
                         in_=w_br[n].rearrange("(kc p) d -> p kc d", p=128)[:, :, dc * 128:(dc + 1) * 128],
                         writes=[f"wG{s_}"], sem=f"wG{s_}")
        load_g(0)
        cnt = 0
        for dc in range(KC):
            if dc + 1 < KC:
                load_g(dc + 1)
            s_ = dc % 2
            for g in range(4):
                gsl = slice(g * 512, (g + 1) * 512)
                for n in range(3):
                    pg, pb = cnt % 2, 2 + cnt % 2
                    sb_ = sgt[cnt % 2]
                    sk = f"sg{cnt % 2}"
                    cnt += 1
                    for kc in range(KC):
                        self.mm(B[pg], wG[s_][:, kc, n, :], xT[:, kc, gsl], kc == 0, kc == KC - 1, reads=[f"wG{s_}"], writes=[f"B{pg}"])
                    for kc in range(4):
                        self.mm(B[pb], wBr[s_][:, kc, n, :], YT[:, n * 4 + kc, gsl], kc == 0, kc == 3, reads=[f"wG{s_}"], writes=[f"B{pb}"])
                    self.actf(sb_, B[pg], AF.Sigmoid, reads=[f"B{pg}", "gbT"], writes=[sk],
                              bias=gbT[:, n * 8 + dc:n * 8 + dc + 1], scale=1.0)
                    if n == 0:
                        self.tt(macc, sb_, B[pb], ALU.mult, reads=[sk, f"B{pb}"], writes=["macc"])
                    elif n == 1:
                        self.tt(mtmp, sb_, B[pb], ALU.mult, reads=[sk, f"B{pb}"], writes=["mtmp"])
                        self.tt(macc, macc, mtmp, ALU.add, reads=["macc", "mtmp"], writes=["macc"])
                    else:
                        self.tt(mtmp, sb_, B[pb], ALU.mult, reads=[sk, f"B{pb}"], writes=["mtmp"])
                        self.tt(mergedT[:, dc, gsl], macc, mtmp, ALU.add, reads=["macc", "mtmp"], writes=[("mT", dc, g)])
        self.barrier()
        if self.stop_after == ("P4", l):
            self.tap("mergedT", mergedT, [128, KC, S])
            return

        self.off = self.MBASE
        acc = self.acc = self.A([128, NT, D], F32)
        self.off = P5_OFF
        moe = (l % 2 == 1)
        wO = self.A([128, KC, D], BF16)
        self.dma(pool, out=wO, in_=self.w_out[l].rearrange("(kc p) n -> p kc n", p=128), writes=["wO"], sem="wA")
        self.dma(sp, out=self.lng, in_=self.ln_gb[l][0:1, :].partition_broadcast(128), writes=["lng"], sem="lnp")
        self.dma(sp, out=self.lnb, in_=self.ln_gb[l][1:2, :].partition_broadcast(128), writes=["lnb"], sem="lnp")
        for r_ in ("lng", "lnb"):
            self.R(r_).w = (self.dsem("lnp"), self.dsem("lnp").cnt)
        xr = [self.A([128, D], F32) for _ in range(2)]
        sres = self.A([128, D], F32)
        xbf = self.A([128, D], BF16)
        xsrc = self.x if l == 0 else self.xres
        if moe:
            wR = self.A([128, KC, NEXP], F32)
            self.dma(sp, out=wR, in_=self.router.rearrange("(kc p) e -> p kc e", p=128), writes=["wR"], sem="small")
            x1T = self.A([128, KC, 128], F32)
            lgt = self.A([128, 8], F32)
            mx8 = self.A([128, 8], F32)
            nm1 = self.A([128, 1], F32)
            msk = self.A([128, 8], F32)
            eg = self.A([128, 8], F32)
            ssum = self.A([128, 1], F32)
        for i in range(NT):
            tsl = slice(i * 128, (i + 1) * 128)
            b = i % 2
            self.dma(sp, out=xr[b], in_=xsrc[tsl, :], reads=[("xres", i)], writes=[f"xr{b}"], sem=f"xr{b}")
            for hf in range(2):
                pb_ = 2 * b + hf
                for kc in range(KC):
                    self.mm(B[pb_], mergedT[:, kc, tsl], wO[:, kc, hf * 512:(hf + 1) * 512], kc == 0, kc == KC - 1,
                            reads=["wO"], writes=[f"B{pb_}"])
            for hf in range(2):
                pb_ = 2 * b + hf
                self.stt(sres[:, hf * 512:(hf + 1) * 512], xr[b][:, hf * 512:(hf + 1) * 512], float(ALPHA), B[pb_],
                         ALU.mult, ALU.add, reads=[f"xr{b}", f"B{pb_}"], writes=["sres"])
            mv, rstd = self.ln_stats(sres, D, LN_EPS, "sres", (self.lnst, self.lnmv, self.lnrstd))
            self.ts(sres, sres, mv[:, 0:1], rstd[:, 0:1], ALU.subtract, ALU.mult, reads=["sres", "ln_mv", "ln_rstd"], writes=["sres"])
            self.tt(sres, sres, self.lng, ALU.mult, reads=["sres", "lng"], writes=["sres"])
            self.tt(sres, sres, self.lnb, ALU.add, reads=["sres", "lnb"], writes=["sres"])
            self.actf(acc[:, i, :], sres, AF.Identity, reads=["sres"], writes=[("acc", i)], scale=float(ALPHA))
            self.actf(xbf, sres, AF.Copy, reads=["sres"], writes=["xbf"])
            psT = Bb[4].rearrange("p (a b) -> p a b", a=8)
            for kc in range(KC):
                self.tr(psT[:, kc, :], xbf[:, kc * 128:(kc + 1) * 128], self.identb, reads=["xbf"], writes=["B4"], inc=(kc == KC - 1))
            self.actf(xT[:, :, tsl], psT, AF.Copy, reads=["B4"], writes=[("xT", i)])
            if moe:
                for kc in range(KC):
                    bk = 5 + kc // 4
                    self.tr(B[bk][:, (kc % 4) * 128:(kc % 4 + 1) * 128], sres[:, kc * 128:(kc + 1) * 128], self.identf,
                            reads=["sres", "cf"], writes=[f"B{bk}"], inc=(kc % 4 == 3))
                for hb in range(2):
                    self.vcopy(x1T[:, hb * 4:(hb + 1) * 4, :].rearrange("p a b -> p (a b)"), B[5 + hb],
                               reads=[f"B{5 + hb}"], writes=["x1T"])
                for kc in range(KC):
                    self.mm(B[7][:, 0:8], x1T[:, kc, :], wR[:, kc, :], kc == 0, kc == KC - 1, reads=["x1T", "wR"], writes=["B7"])
                self.vcopy(lgt, B[7][:, 0:8], reads=["B7"], writes=["lgt"])
                self.op(dve, lambda: nc.vector.max(out=mx8, in_=lgt), reads=["lgt"], writes=["mx8"])
                self.ts(nm1, mx8[:, 0:1], -1.0, None, ALU.mult, reads=["mx8"], writes=["nm1"])
                self.ts(msk, lgt, mx8[:, 1:2], None, ALU.is_ge, reads=["lgt", "mx8"], writes=["msk"])
                self.actf(eg, lgt, AF.Exp, reads=["lgt", "nm1"], writes=["eg"], bias=nm1[:, 0:1], scale=1.0)
                self.tt(eg, eg, msk, ALU.mult, reads=["eg", "msk"], writes=["eg"])
                self.op(dve, lambda: nc.vector.reduce_sum(out=ssum, in_=eg, axis=mybir.AxisListType.X), reads=["eg"], writes=["ssum"])
                self.op(dve, lambda: nc.vector.reciprocal(out=ssum, in_=ssum), reads=["ssum"], writes=["ssum"])
                self.ts(self.gates[:, i, :], eg, ssum[:, 0:1], None, ALU.mult, reads=["eg", "ssum"], writes=[("gates", i)])
        self.barrier()
        if self.stop_after == ("P5", l):
            self.tap("acc", acc, [128, NT, D])
            self.tap("gates", self.gates, [128, NT, NEXP])
            return

    def ffn(self, l):
        nc = self.nc
        pe, act, dve, pool, sp = self.pe, self.act, self.dve, self.pool, self.sp
        xT = self.xT
        moe = (l % 2 == 1)
        last = (l == self.n_layers - 1)
        B = [self.bank(b) for b in range(8)]
        Bb = [self.bank(b, BF16) for b in range(8)]
        self.off = self.MBASE
        acc = self.A([128, NT, D], F32)
        hT = self.A([128, 4, S], BF16)
        upb = [self.A([128, KC, 2, 512], BF16) for _ in range(2)]
        dnb = [self.A([128, 4, D], BF16) for _ in range(2)]
        sa = [self.A([128, 512], F32) for _ in range(2)]
        if moe:
            F = FF_EXP
            segs = [(e, f0, 4) for e in range(NEXP) for f0 in range(0, F // 128, 4)]
        else:
            F = FF_DENSE
            nch = F // 128
            segs = [(0, f0, min(4, nch - f0)) for f0 in range(0, nch, 4)]

        def wup(e):
            t = self.moe_up[e] if moe else self.dense_up
            return t.rearrange("(kc p) n -> p kc n", p=128)

        def wdn(e):
            t = self.moe_down[e] if moe else self.dense_down
            return t.rearrange("(fc p) d -> p fc d", p=128)

        def load(q):
            e, f0, nch = segs[q]
            s_ = q % 2
            w = nch * 128
            self.dma(pool, out=upb[s_][:, :, 0, 0:w], in_=wup(e)[:, :, f0 * 128:f0 * 128 + w], writes=[f"up{s_}"], sem=f"up{s_}")
            self.dma(pool, out=upb[s_][:, :, 1, 0:w], in_=wup(e)[:, :, F + f0 * 128:F + f0 * 128 + w], writes=[f"up{s_}"], sem=f"up{s_}")
            self.dma(pool, out=dnb[s_][:, 0:nch, :], in_=wdn(e)[:, f0:f0 + nch, :], writes=[f"dn{s_}"], sem=f"dn{s_}")

        load(0)
        ucnt = 0
        ocnt = 0
        for q, (e, f0, nch) in enumerate(segs):
            if q + 1 < len(segs):
                load(q + 1)
            s_ = q % 2
            for fc in range(nch):
                for g in range(4):
                    gsl = slice(g * 512, (g + 1) * 512)
                    pa, pb = ucnt % 2, 2 + ucnt % 2
                    sab = sa[ucnt % 2]
                    sk = f"sa{ucnt % 2}"
                    ucnt += 1
                    for kc in range(KC):
                        self.mm(B[pa], upb[s_][:, kc, 0, fc * 128:(fc + 1) * 128], xT[:, kc, gsl], kc == 0, kc == KC - 1,
                                reads=[f"up{s_}"], writes=[f"B{pa}"])
                    for kc in range(KC):
                        self.mm(B[pb], upb[s_][:, kc, 1, fc * 128:(fc + 1) * 128], xT[:, kc, gsl], kc == 0, kc == KC - 1,
                                reads=[f"up{s_}"], writes=[f"B{pb}"])
                    self.actf(sab, B[pa], AF.Silu, reads=[f"B{pa}"], writes=[sk])
                    self.tt(hT[:, fc, gsl], sab, B[pb], ALU.mult, reads=[sk, f"B{pb}"], writes=[("hT", fc, g)])
            for i in range(NT):
                tsl = slice(i * 128, (i + 1) * 128)
                for hf in range(2):
                    po = 4 + ocnt % 4
                    ocnt += 1
                    for fc in range(nch):
                        self.mm(B[po], hT[:, fc, tsl], dnb[s_][:, fc, hf * 512:(hf + 1) * 512], fc == 0, fc == nch - 1,
                                reads=[("hT", fc, i // 4), f"dn{s_}"], writes=[f"B{po}"])
                    asl = acc[:, i, hf * 512:(hf + 1) * 512]
                    if moe:
                        self.stt(asl, B[po], self.gates[:, i, e:e + 1], asl, ALU.mult, ALU.add,
                                 reads=[f"B{po}", ("acc", i, hf)], writes=[("acc", i, hf)])
                    else:
                        self.tt(asl, B[po], asl, ALU.add, reads=[f"B{po}", ("acc", i, hf)], writes=[("acc", i, hf)])
        self.barrier()
        if self.stop_after == ("F1", l):
            self.tap("acc", acc, [128, NT, D])
            return
        self.off = self.MBASE + 65536
        self.dma(sp, out=self.lng, in_=self.ln_gb[l][2:3, :].partition_broadcast(128), writes=["lng"], sem="lnp")
        self.dma(sp, out=self.lnb, in_=self.ln_gb[l][3:4, :].partition_broadcast(128), writes=["lnb"], sem="lnp")
        for r_ in ("lng", "lnb"):
            self.R(r_).w = (self.dsem("lnp"), self.dsem("lnp").cnt)
        xo = [self.A([128, D], F32) for _ in range(2)]
        xbf = self.A([128, D], BF16)
        dst = self.y if last else self.xres
        for i in range(NT):
            tsl = slice(i * 128, (i + 1) * 128)
            b = i % 2
            ak = [("acc", i, 0), ("acc", i, 1)]
            mv, rstd = self.ln_stats(acc[:, i, :], D, LN_EPS, ("acc", i, 0), (self.lnst, self.lnmv, self.lnrstd))
            self.ts(xo[b], acc[:, i, :], mv[:, 0:1], rstd[:, 0:1], ALU.subtract, ALU.mult,
                    reads=ak + ["ln_mv", "ln_rstd"], writes=[f"xo{b}"])
            self.tt(xo[b], xo[b], self.lng, ALU.mult, reads=[f"xo{b}", "lng"], writes=[f"xo{b}"])
            self.tt(xo[b], xo[b], self.lnb, ALU.add, reads=[f"xo{b}", "lnb"], writes=[f"xo{b}"])
            self.dma(sp, out=dst[tsl, :], in_=xo[b], reads=[f"xo{b}"], writes=[("xres", i)], sem=f"xo{b}")
            if not last:
                self.actf(xbf, xo[b], AF.Copy, reads=[f"xo{b}"], writes=["xbf"])
                psT = Bb[i % 2].rearrange("p (a b) -> p a b", a=8)
                for kc in range(KC):
                    self.tr(psT[:, kc, :], xbf[:, kc * 128:(kc + 1) * 128], self.identb, reads=["xbf"], writes=[f"B{i % 2}"],
                            inc=(kc == KC - 1))
                self.actf(xT[:, :, tsl], psT, AF.Copy, reads=[f"B{i % 2}"], writes=[("xT", i)])
        self.barrier()


def prep_inputs(inputs, n_layers=2):
    f = lambda a: np.ascontiguousarray(np.asarray(a, dtype=np.float32))
    cf, cb = host_consts()
    shared = {"constf": cf, "constb": cb}
    for l in range(n_layers):
        shared[f"w_in{l}"] = f(inputs["w_in"][l])
        shared[f"fox_b{l}"] = f(np.asarray(inputs["fox_b_f"])[l].reshape(1, 8))
        shared[f"gate_bT{l}"] = f(np.asarray(inputs["gate_b"])[l].reshape(24, 128).T)
        shared[f"gm_wsT{l}"] = f(np.asarray(inputs["gm_w_s"])[l].transpose(2, 0, 1).reshape(128, 512))
        shared[f"gm_bsT{l}"] = f(np.asarray(inputs["gm_b_s"])[l].T)
        shared[f"gm_ln{l}"] = f(np.stack([np.asarray(inputs["gm_ln_g"])[l], np.asarray(inputs["gm_ln_b"])[l]]))
        shared[f"w_branch{l}"] = f(inputs["w_branch"][l])
        shared[f"w_out{l}"] = f(inputs["w_out"][l])
        lg, lb = np.asarray(inputs["ln_g"])[l], np.asarray(inputs["ln_b"])[l]
        shared[f"ln_gb{l}"] = f(np.stack([lg[0], lb[0], lg[1], lb[1]]))
    shared["dense_up"] = f(inputs["dense_w_up"][0])
    shared["dense_down"] = f(inputs["dense_w_down"][0])
    if n_layers > 1:
        shared["router"] = f(inputs["moe_router"][0])
        shared["moe_up"] = f(inputs["moe_w_up"][0])
        shared["moe_down"] = f(inputs["moe_w_down"][0])
    return shared


def kernel(**inputs):
    x = np.asarray(inputs["x"], dtype=np.float32)
    nb = x.shape[0]
    kb = KB(n_layers=DEPTH)
    nc = kb.build()
    shared = prep_inputs(inputs, DEPTH)
    in_maps = []
    for b in range(nb):
        m = dict(shared)
        m["x"] = np.ascontiguousarray(x[b])
        in_maps.append(m)
    res = run_bass_kernel_spmd(nc, in_maps, core_ids=list(range(nb)))
    out = np.stack([np.asarray(res.results[b]["y"], dtype=np.float32) for b in range(nb)], axis=0)
    return out
```

```python
import numpy as np
import concourse.bass as bass
import concourse.mybir as mybir
from concourse.bass_utils import run_bass_kernel_spmd

F32 = mybir.dt.float32
BF16 = mybir.dt.bfloat16
U8 = mybir.dt.uint8
AF = mybir.ActivationFunctionType
ALU = mybir.AluOpType

S = 2048
D = 1024
NT = 16
KC = 8
N_IN = 7176
DEPTH = 2
FF_DENSE = 2816
FF_EXP = 3584
NEXP = 8
ALPHA = (2 * DEPTH) ** 0.25
LN_EPS = 1e-5
GN_EPS = 1e-6
NEG = -30000.0

CF_COS, CF_SIN, CF_DECIN, CF_KDM, CF_QDEC, CF_SMALL, CF_TRI, CF_IDENT, CF_NEGM, CF_ONES = (
    0, 512, 1024, 1536, 2048, 2304, 2308, 2436, 2564, 2692)
NCF1 = 2304
NCF = 2820
CB_HMASK, CB_MASK, CB_IDENT = 0, 1024, 1152
NCB = 1280


def host_consts():
    lg = np.log1p(-np.exp2(-5.0 - np.arange(4, dtype=np.float64)))
    p = np.arange(128)
    cf = np.zeros((128, NCF), np.float32)
    pos = np.arange(S, dtype=np.float32)
    inv_freq = (10000.0 ** (-np.arange(32, dtype=np.float32) / 32)).astype(np.float32)
    ang = (pos[:, None] * inv_freq[None, :]).astype(np.float32)
    cos = np.cos(ang).astype(np.float32).reshape(NT, 128, 32).transpose(1, 0, 2).reshape(128, 512)
    sin = np.sin(ang).astype(np.float32).reshape(NT, 128, 32).transpose(1, 0, 2).reshape(128, 512)
    cf[:, CF_COS:CF_COS + 512] = cos
    cf[:, CF_SIN:CF_SIN + 512] = sin
    j = p[:, None]
    i = p[None, :]
    decin = np.zeros((128, 4, 128))
    kdm = np.zeros((128, 4, 128))
    qdec = np.zeros((128, 4, 64))
    for h in range(4):
        decin[:, h, :] = np.where(i >= j, np.exp(lg[h] * np.maximum(i - j, 0)), 0.0) * 0.125
        hl = h % 2
        kdm[:, h, hl * 64:(hl + 1) * 64] = (np.exp(lg[h] * (127 - p)) * 0.125)[:, None]
        qdec[:, h, :] = np.exp(lg[h] * (p + 1.0))[:, None]
    cf[:, CF_DECIN:CF_DECIN + 512] = decin.reshape(128, 512)
    cf[:, CF_KDM:CF_KDM + 512] = kdm.reshape(128, 512)
    cf[:, CF_QDEC:CF_QDEC + 256] = qdec.reshape(128, 256)
    cf[:, CF_SMALL + 0] = (p < 64)
    cf[:, CF_SMALL + 1] = (p >= 64)
    for pr in range(2):
        cf[:, CF_SMALL + 2 + pr] = np.exp(np.where(p < 64, lg[2 * pr], lg[2 * pr + 1]) * 128.0)
    tri = (j <= i).astype(np.float32)
    cf[:, CF_TRI:CF_TRI + 128] = tri
    cf[:, CF_IDENT:CF_IDENT + 128] = np.eye(128, dtype=np.float32)
    cf[:, CF_NEGM:CF_NEGM + 128] = np.where(j <= i, 0.0, NEG)
    cf[:, CF_ONES:CF_ONES + 128] = 1.0
    cb = np.zeros((128, NCB), np.float32)
    hm = np.zeros((128, 8, 128), np.float32)
    for h in range(8):
        hm[h * 6:(h + 1) * 6, h, :] = 1.0
    cb[:, CB_HMASK:CB_HMASK + 1024] = hm.reshape(128, 1024)
    cb[:, CB_MASK:CB_MASK + 128] = tri
    cb[:, CB_IDENT:CB_IDENT + 128] = np.eye(128, dtype=np.float32)
    return cf, cb


class Eng:
    def __init__(self, nc, eng, name):
        self.eng = eng
        self.sem = nc.alloc_semaphore("s_" + name)
        self.cnt = 0
        self.seen = {}
        self.name = name


class DSem:
    def __init__(self, nc, name):
        self.sem = nc.alloc_semaphore("d_" + name)
        self.cnt = 0
        self.name = name


class Res:
    __slots__ = ("w", "r")

    def __init__(self):
        self.w = None
        self.r = {}


class Cut(Exception):
    pass


class KB:
    def ck(self, n):
        if self.cutn == n:
            raise Cut()

    def __init__(self, n_layers=2, debug=False, stop_after=None, cutn=None):
        self.cutn = cutn
        self.n_layers = n_layers
        self.debug = debug
        self.stop_after = stop_after
        self.taps = []
        nc = self.nc = bass.Bass("TRN2", target_bir_lowering=False)
        self.pe = Eng(nc, nc.tensor, "pe")
        self.act = Eng(nc, nc.scalar, "act")
        self.dve = Eng(nc, nc.vector, "dve")
        self.pool = Eng(nc, nc.gpsimd, "pool")
        self.sp = Eng(nc, nc.sync, "sp")
        self.engs = [self.pe, self.act, self.dve, self.pool, self.sp]
        self.dsems = {}
        self.pending = None
        self.res = {}
        self.inputs = {}
        di = self.din
        self.x = di("x", [S, D])
        self.constf = di("constf", [128, NCF])
        self.constb = di("constb", [128, NCB])
        L = n_layers
        self.w_in = [di(f"w_in{l}", [D, N_IN]) for l in range(L)]
        self.fox_b = [di(f"fox_b{l}", [1, 8]) for l in range(L)]
        self.gate_bT = [di(f"gate_bT{l}", [128, 24]) for l in range(L)]
        self.gm_wsT = [di(f"gm_wsT{l}", [128, 512]) for l in range(L)]
        self.gm_bsT = [di(f"gm_bsT{l}", [128, 4]) for l in range(L)]
        self.gm_ln = [di(f"gm_ln{l}", [2, 512]) for l in range(L)]
        self.w_branch = [di(f"w_branch{l}", [3, 512, D]) for l in range(L)]
        self.w_out = [di(f"w_out{l}", [D, D]) for l in range(L)]
        self.ln_gb = [di(f"ln_gb{l}", [4, D]) for l in range(L)]
        self.dense_up = di("dense_up", [D, 2 * FF_DENSE])
        self.dense_down = di("dense_down", [FF_DENSE, D])
        if L > 1:
            self.router = di("router", [D, NEXP])
            self.moe_up = di("moe_up", [NEXP, D, 2 * FF_EXP])
            self.moe_down = di("moe_down", [NEXP, FF_EXP, D])
        self.y = nc.dram_tensor("y", [S, D], F32, kind="ExternalOutput").ap()
        self.xres = nc.dram_tensor("xres", [S, D], F32).ap()
        nbytes = (int(nc.sbuf_bytes_remaining) // 64) * 64 - 64
        self.arena_bytes = nbytes
        self.arena = nc.alloc_sbuf_tensor("arena", [128, nbytes], U8)
        self.psum = nc.alloc_psum_tensor("psum", [128, 4096], F32)
        self.off = 0

    def din(self, name, shape):
        t = self.nc.dram_tensor(name, list(shape), F32, kind="ExternalInput").ap()
        self.inputs[name] = tuple(shape)
        return t

    def A(self, shape, dt):
        esz = 4 if dt == F32 else 2
        n = int(np.prod(shape[1:]))
        nb = n * esz
        off = self.off
        assert off + nb <= self.arena_bytes, ("SBUF arena overflow", off, nb, self.arena_bytes)
        ap = self.arena[:, off:off + nb].bitcast(dt)
        if len(shape) == 3:
            ap = ap.rearrange("p (a b) -> p a b", a=shape[1])
        elif len(shape) == 4:
            ap = ap.rearrange("p (a b c) -> p a b c", a=shape[1], b=shape[2])
        self.off = off + ((nb + 63) // 64) * 64
        return ap

    def Vat(self, off, shape, dt):
        save = self.off
        self.off = off
        ap = self.A(shape, dt)
        self.off = save
        return ap

    def prefetch_AB(self, l):
        w_in = self.w_in[l].rearrange("(kc p) n -> p kc n", p=128)
        wA = self.Vat(self.SLOT[0], [128, KC, 1536], BF16)
        wB = self.Vat(self.SLOT[1], [128, KC, 1544], BF16)
        self.dma(self.pool, out=wA, in_=w_in[:, :, 0:1536], writes=["slot0"], sem="slot0")
        self.dma(self.pool, out=wB, in_=w_in[:, :, 1536:3080], writes=["slot1"], sem="slot1")

    def bank(self, b, dt=F32):
        ap = self.psum[:, b * 512:(b + 1) * 512]
        if dt == BF16:
            ap = ap.bitcast(BF16)
        return ap

    def R(self, k):
        r = self.res.get(k)
        if r is None:
            r = self.res[k] = Res()
        return r

    def dsem(self, name):
        s = self.dsems.get(name)
        if s is None:
            s = self.dsems[name] = DSem(self.nc, name)
        return s

    def wait(self, e, so, v):
        if v <= 0 or e.seen.get(so, 0) >= v:
            return
        if self.pending is not None:
            cur = self.pending.get(so, 0)
            if v > cur:
                self.pending[so] = v
            return
        e.eng.wait_ge(so.sem, v)
        e.seen[so] = v

    def flush_waits(self, e, keep_one):
        items = [(so, v) for so, v in self.pending.items() if e.seen.get(so, 0) < v]
        self.pending = None
        last = None
        if keep_one and items:
            last = items.pop()
        for so, v in items:
            e.eng.wait_ge(so.sem, v)
            e.seen[so] = v
        return last

    def _deps(self, e, reads, writes):
        for k in reads:
            R = self.R(k)
            if R.w is not None:
                so, v = R.w
                if not (so is e and e is self.pe):
                    self.wait(e, so, v)
        for k in writes:
            R = self.R(k)
            if R.w is not None:
                so, v = R.w
                if so is not e:
                    self.wait(e, so, v)
            for so, v in R.r.items():
                if so is not e:
                    self.wait(e, so, v)

    def op(self, e, fn, reads=(), writes=(), inc=True):
        if e is not self.pe:
            extra = [k for k in reads if isinstance(k, str) and k[0] == "B" and k[1:].isdigit() and k not in writes]
            if extra:
                writes = list(writes) + extra
        self.pending = {}
        self._deps(e, reads, writes)
        last = self.flush_waits(e, keep_one=(e is not self.pe))
        ins = fn()
        if last is not None:
            ins._wait_ge(last[0].sem, last[1])
            e.seen[last[0]] = last[1]
        if inc:
            ins.then_inc(e.sem, 1)
            e.cnt += 1
            tag = e.cnt
        else:
            tag = e.cnt + 1
        for k in reads:
            R = self.R(k)
            R.r[e] = max(R.r.get(e, 0), tag)
        for k in writes:
            R = self.R(k)
            R.w = (e, tag)
            R.r = {}
        return ins

    def dma(self, q, out, in_, reads=(), writes=(), sem="misc", **kw):
        Sm = self.dsem(sem)
        self._deps(q, reads, writes)
        q.eng.dma_start(out=out, in_=in_, **kw).then_inc(Sm.sem, 16)
        Sm.cnt += 16
        for k in reads:
            self.R(k).r[Sm] = Sm.cnt
        for k in writes:
            R = self.R(k)
            R.w = (Sm, Sm.cnt)
            R.r = {}

    def barrier(self):
        for e in self.engs:
            for o in self.engs:
                if o is not e:
                    self.wait(e, o, o.cnt)
            for s in self.dsems.values():
                self.wait(e, s, s.cnt)

    def tap(self, name, ap, shape, reads=()):
        if not self.debug:
            return
        t = self.nc.dram_tensor("dbg_" + name, list(shape), ap.dtype, kind="ExternalOutput").ap()
        self.dma(self.sp, out=t, in_=ap, reads=reads, sem="dbg")
        self.taps.append("dbg_" + name)

    def mm(self, out, lhsT, rhs, start, stop, reads=(), writes=(), inc=None):
        if inc is None:
            inc = stop
        return self.op(self.pe, lambda: self.nc.tensor.matmul(out, lhsT=lhsT, rhs=rhs, start=start, stop=stop),
                       reads, writes, inc)

    def tr(self, out, in_, ident, reads=(), writes=(), inc=True):
        return self.op(self.pe, lambda: self.nc.tensor.transpose(out=out, in_=in_, identity=ident),
                       reads, writes, inc)

    def actf(self, out, in_, func, reads=(), writes=(), **kw):
        return self.op(self.act, lambda: self.nc.scalar.activation(out=out, in_=in_, func=func, **kw), reads, writes)

    def tt(self, out, in0, in1, op, reads=(), writes=()):
        return self.op(self.dve, lambda: self.nc.vector.tensor_tensor(out=out, in0=in0, in1=in1, op=op), reads, writes)

    def ts(self, out, in0, s1, s2, op0, op1=None, reads=(), writes=()):
        if op1 is None:
            return self.op(self.dve, lambda: self.nc.vector.tensor_scalar(out=out, in0=in0, scalar1=s1, scalar2=None, op0=op0),
                           reads, writes)
        return self.op(self.dve, lambda: self.nc.vector.tensor_scalar(out=out, in0=in0, scalar1=s1, scalar2=s2, op0=op0, op1=op1),
                       reads, writes)

    def stt(self, out, in0, scalar, in1, op0, op1, reads=(), writes=()):
        return self.op(self.dve, lambda: self.nc.vector.scalar_tensor_tensor(out=out, in0=in0, scalar=scalar, in1=in1, op0=op0, op1=op1),
                       reads, writes)

    def vcopy(self, out, in_, reads=(), writes=()):
        return self.op(self.dve, lambda: self.nc.vector.tensor_copy(out=out, in_=in_), reads, writes)

    def ln_stats(self, src, n, eps, key_src, tmp, ks=""):
        st, mv, rstd = tmp
        nch = (n + 511) // 512
        for c in range(nch):
            w = min(512, n - c * 512)
            self.op(self.dve, lambda c=c, w=w: self.nc.vector.bn_stats(out=st[:, c, :], in_=src[:, c * 512:c * 512 + w]),
                    reads=[key_src], writes=["ln_st" + ks])
        self.op(self.dve, lambda: self.nc.vector.bn_aggr(out=mv, in_=st[:, 0:nch, :].rearrange("p a b -> p (a b)")),
                reads=["ln_st" + ks], writes=["ln_mv" + ks])
        self.actf(rstd, mv[:, 1:2], AF.Sqrt, reads=["ln_mv" + ks], writes=["ln_rstd" + ks], bias=eps, scale=1.0)
        self.op(self.dve, lambda: self.nc.vector.reciprocal(out=rstd, in_=rstd), reads=["ln_rstd" + ks], writes=["ln_rstd" + ks])
        return mv, rstd

    def interleave(self, genf, n, width=2, lag=3):
        active = []
        nxt = 0
        while nxt < n or active:
            if nxt < n and len(active) < width and (not active or active[-1][1] >= lag):
                active.append([genf(nxt), 0])
                nxt += 1
            for a in list(active):
                try:
                    next(a[0])
                    a[1] += 1
                except StopIteration:
                    active.remove(a)

    def build(self):
        nc = self.nc
        pe, act, dve, pool, sp = self.pe, self.act, self.dve, self.pool, self.sp
        self.xT = self.A([128, KC, S], BF16)
        self.cf = self.A([128, NCF - NCF1], F32)
        self.cb = self.A([128, NCB], BF16)
        cf, cb = self.cf, self.cb
        o_ = NCF1
        self.pmask = cf[:, CF_SMALL - o_:CF_SMALL - o_ + 2]
        self.sdec = cf[:, CF_SMALL - o_ + 2:CF_SMALL - o_ + 4]
        self.tri = cf[:, CF_TRI - o_:CF_TRI - o_ + 128]
        self.identf = cf[:, CF_IDENT - o_:CF_IDENT - o_ + 128]
        self.negm = cf[:, CF_NEGM - o_:CF_NEGM - o_ + 128]
        self.onesf = cf[:, CF_ONES - o_:CF_ONES - o_ + 128]
        self.hmask = cb[:, CB_HMASK:CB_HMASK + 1024].rearrange("p (a b) -> p a b", a=8)
        self.mask01 = cb[:, CB_MASK:CB_MASK + 128]
        self.identb = cb[:, CB_IDENT:CB_IDENT + 128]
        self.lng = self.A([128, D], F32)
        self.lnb = self.A([128, D], F32)
        self.gates = self.A([128, NT, NEXP], F32)
        self.st = self.A([128, 4, 6], F32)
        self.mv = self.A([128, 4, 2], F32)
        self.rstd = self.A([128, 4], F32)
        self.lnst = self.A([128, 2, 6], F32)
        self.lnmv = self.A([128, 2], F32)
        self.lnrstd = self.A([128, 1], F32)
        self.lnt = [(self.A([128, 2, 6], F32), self.A([128, 2], F32), self.A([128, 1], F32)) for _ in range(2)]
        self.gnt = [(self.A([128, 4, 6], F32), self.A([128, 4, 2], F32), self.A([128, 4], F32)) for _ in range(2)]
        self.MBASE = self.off
        self.SLOT = [self.MBASE + 107520, self.MBASE + 107520 + 24704]
        assert self.SLOT[1] + 24704 <= self.arena_bytes, (self.MBASE, self.arena_bytes)
        self.dma(sp, out=cf, in_=self.constf[:, NCF1:NCF], writes=["cf"], sem="const")
        self.dma(pool, out=cb, in_=self.constb[:, :], writes=["cb"], sem="const")

        self.off = self.MBASE
        xbs = [self.A([128, D], BF16) for _ in range(2)]
        for i in range(NT):
            b = i % 2
            self.dma(pool, out=xbs[b], in_=self.x[i * 128:(i + 1) * 128, :], writes=[f"xb{b}"], sem=f"xb{b}")
            psT = self.bank(b, BF16).rearrange("p (a b) -> p a b", a=8)
            for kc in range(KC):
                self.tr(psT[:, kc, :], xbs[b][:, kc * 128:(kc + 1) * 128], self.identb,
                        reads=[f"xb{b}", "cb"], writes=[f"ps{b}"], inc=(kc == KC - 1))
            self.actf(self.xT[:, :, i * 128:(i + 1) * 128], psT, AF.Copy, reads=[f"ps{b}"], writes=[("xT", i)])
        self.barrier()
        if self.stop_after == ("P0", 0):
            self.tap("xT", self.xT, [128, KC, S])
            self.barrier()
            return nc

        for l in range(self.n_layers):
            try:
                self.mixer(l)
            except Cut:
                self.barrier()
                return nc
            if self.stop_after is not None and self.stop_after[0] in ("P1", "P2", "P3", "P4", "P5") and self.stop_after[1] == l:
                break
            if self.stop_after == ("mixer", l):
                break
            self.ffn(l)
            if self.stop_after == ("ffn", l):
                break
        self.barrier()
        return nc

    def mixer(self, l):
        nc = self.nc
        pe, act, dve, pool, sp = self.pe, self.act, self.dve, self.pool, self.sp
        xT = self.xT
        w_in = self.w_in[l].rearrange("(kc p) n -> p kc n", p=128)
        self.off = self.MBASE
        YT = self.A([128, 12, S], BF16)
        P_OFF = self.off
        B = [self.bank(b) for b in range(8)]
        Bb = [self.bank(b, BF16) for b in range(8)]

        if l == 0:
            self.prefetch_AB(0)
        wA = self.Vat(self.SLOT[0], [128, KC, 1536], BF16)
        cf1 = self.A([128, NCF1], F32)
        self.dma(sp, out=cf1, in_=self.constf[:, 0:NCF1], writes=["cf1"], sem="cf1")
        self.cosv = cf1[:, CF_COS:CF_COS + 512].rearrange("p (a b) -> p a b", a=NT)
        self.sinv = cf1[:, CF_SIN:CF_SIN + 512].rearrange("p (a b) -> p a b", a=NT)
        self.decin = cf1[:, CF_DECIN:CF_DECIN + 512]
        self.kdm = cf1[:, CF_KDM:CF_KDM + 512].rearrange("p (a b) -> p a b", a=4)
        self.qdec = cf1[:, CF_QDEC:CF_QDEC + 256]

        def p1set():
            return dict(rot=self.A([128, 8, 2, 32], F32), t1=self.A([128, 8, 32], F32), t2=self.A([128, 8, 32], F32),
                        rot_bf=self.A([128, 512], BF16), qp_bf=self.A([128, 256], BF16), kpm=self.A([128, 4, 128], BF16),
                        qTm=self.A([128, 4, 128], BF16), qpTm=self.A([128, 4, 128], BF16), kT=self.A([128, 2, 128], BF16),
                        ST=self.A([128, 4, 128], BF16), v_bf=self.A([128, 512], BF16), yn=self.A([128, 512], F32),
                        sg=self.A([128, 512], F32), ya=self.A([128, 512], BF16))
        p1s = [p1set(), p1set()]
        assert self.off <= self.SLOT[0], ("P1 locals overlap weight slots", self.off, self.SLOT)
        Sst = self.A([128, 2, 128], F32)
        Sbf = self.A([128, 2, 128], BF16)
        self.op(dve, lambda: nc.vector.memset(Sst, 0.0), writes=["Sst"])
        self.op(dve, lambda: nc.vector.memset(Sbf, 0.0), writes=["Sbf"])
        p1_done = {"n": 0}

        def p1gen(i):
            tsl = slice(i * 128, (i + 1) * 128)
            par = i % 2
            T = p1s[par]
            k = lambda nm: f"{nm}{par}"
            X = [4 * par + j for j in range(4)]
            bk = lambda j: f"B{X[j]}"
            rot, t1, t2, rot_bf, qp_bf, kpm = T["rot"], T["t1"], T["t2"], T["rot_bf"], T["qp_bf"], T["kpm"]
            qTm, qpTm, kT, ST, v_bf, yn, sg, ya = T["qTm"], T["qpTm"], T["kT"], T["ST"], T["v_bf"], T["yn"], T["sg"], T["ya"]
            gst, gmv, grs = self.gnt[par]
            for c in range(3):
                for kc in range(KC):
                    self.mm(B[X[c]], xT[:, kc, tsl], wA[:, kc, c * 512:(c + 1) * 512], kc == 0, kc == KC - 1,
                            reads=["slot0"], writes=[bk(c)])
            yield
            qk = B[X[0]].rearrange("p (g h d) -> p g h d", g=8, h=2)
            x1, x2 = qk[:, :, 0, :], qk[:, :, 1, :]
            cb_ = self.cosv[:, i, :].unsqueeze(1).broadcast_to([128, 8, 32])
            sb_ = self.sinv[:, i, :].unsqueeze(1).broadcast_to([128, 8, 32])
            self.tt(t1, x1, cb_, ALU.mult, reads=[bk(0), "cf1"], writes=[k("t1")])
            self.tt(t2, x2, sb_, ALU.mult, reads=[bk(0)], writes=[k("t2")])
            self.actf(v_bf, B[X[1]], AF.Copy, reads=[bk(1)], writes=[k("v_bf")])
            yield
            self.tt(rot[:, :, 0, :], t1, t2, ALU.subtract, reads=[k("t1"), k("t2")], writes=[k("rot0")])
            self.tt(t1, x1, sb_, ALU.mult, reads=[bk(0)], writes=[k("t1")])
            self.actf(sg, B[X[2]], AF.Silu, reads=[bk(2)], writes=[k("sg")])
            yield
            self.tt(t2, x2, cb_, ALU.mult, reads=[bk(0)], writes=[k("t2")])
            self.tt(rot[:, :, 1, :], t1, t2, ALU.add, reads=[k("t1"), k("t2")], writes=[k("rot1")])
            yield
            rotf = rot.rearrange("p g h d -> p (g h d)")
            self.actf(rot_bf, rotf, AF.Copy, reads=[k("rot0"), k("rot1")], writes=[k("rot_bf")])
            self.tt(qp_bf, rotf[:, 0:256], self.qdec, ALU.mult, reads=[k("rot0"), k("rot1")], writes=[k("qp_bf")])
            yield
            for h in range(4):
                pr = h // 2
                self.tt(kpm[:, h, :], rotf[:, 256 + pr * 128:256 + (pr + 1) * 128], self.kdm[:, h, :], ALU.mult,
                        reads=[k("rot0"), k("rot1")], writes=[k("kpm")])
            yield
            psT = Bb[X[3]].rearrange("p (a b) -> p a b", a=8)
            srcs = [rot_bf[:, 0:128], rot_bf[:, 128:256], rot_bf[:, 256:384], rot_bf[:, 384:512],
                    qp_bf[:, 0:128], qp_bf[:, 128:256]]
            for k_, s_ in enumerate(srcs):
                self.tr(psT[:, k_, :], s_, self.identb, reads=[k("rot_bf"), k("qp_bf"), "cb"], writes=[bk(3)], inc=(k_ == 5))
            yield
            qTm4 = qTm.rearrange("p (a b) t -> p a b t", a=2)
            qpTm4 = qpTm.rearrange("p (a b) t -> p a b t", a=2)
            for pr_ in range(2):
                self.ts(qTm4[:, :, pr_, :], psT[:, 0:2, :], self.pmask[:, pr_:pr_ + 1], None, ALU.mult,
                        reads=[bk(3)], writes=[k("qTm")])
            self.actf(kT, psT[:, 2:4, :], AF.Copy, reads=[bk(3)], writes=[k("kT")])
            yield
            for pr_ in range(2):
                self.ts(qpTm4[:, :, pr_, :], psT[:, 4:6, :], self.pmask[:, pr_:pr_ + 1], None, ALU.mult,
                        reads=[bk(3)], writes=[k("qpTm")])
            yield
            for h in range(4):
                self.mm(B[X[0]][:, h * 128:(h + 1) * 128], kT[:, h // 2, :], qTm[:, h, :], True, True,
                        reads=[k("kT"), k("qTm")], writes=[bk(0)], inc=(h == 3))
            yield
            self.tt(ST.rearrange("p a b -> p (a b)"), B[X[0]], self.decin, ALU.mult, reads=[bk(0)], writes=[k("ST")])
            yield
            while p1_done["n"] < i:
                yield
            for h in range(4):
                self.mm(B[X[1]][:, h * 128:(h + 1) * 128], ST[:, h, :], v_bf[:, h * 128:(h + 1) * 128], True, False,
                        reads=[k("ST"), k("v_bf")], writes=[bk(1)], inc=False)
                self.mm(B[X[1]][:, h * 128:(h + 1) * 128], qpTm[:, h, :], Sbf[:, h // 2, :], False, True,
                        reads=[k("qpTm"), "Sbf"], writes=[bk(1)], inc=(h == 3))
            for pr in range(2):
                for hl in range(2):
                    h = 2 * pr + hl
                    self.mm(B[X[2]][:, pr * 128:(pr + 1) * 128], kpm[:, h, :], v_bf[:, h * 128:(h + 1) * 128],
                            hl == 0, hl == 1, reads=[k("kpm"), k("v_bf")], writes=[bk(2)], inc=(pr == 1 and hl == 1))
            yield
            for pr in range(2):
                self.stt(Sst[:, pr, :], Sst[:, pr, :], self.sdec[:, pr:pr + 1], B[X[2]][:, pr * 128:(pr + 1) * 128],
                         ALU.mult, ALU.add, reads=["Sst", bk(2)], writes=["Sst"])
            self.actf(Sbf, Sst, AF.Copy, reads=["Sst"], writes=["Sbf"])
            p1_done["n"] = i + 1
            yield
            for h in range(4):
                self.op(dve, lambda h=h: nc.vector.bn_stats(out=gst[:, h, :], in_=B[X[1]][:, h * 128:(h + 1) * 128]),
                        reads=[bk(1)], writes=[k("gst")])
            yield
            for h in range(4):
                self.op(dve, lambda h=h: nc.vector.bn_aggr(out=gmv[:, h, :], in_=gst[:, h, :]),
                        reads=[k("gst")], writes=[k("gmv")])
            self.actf(grs, gmv[:, :, 1], AF.Sqrt, reads=[k("gmv")], writes=[k("grs")], bias=GN_EPS, scale=1.0)
            yield
            self.op(dve, lambda: nc.vector.reciprocal(out=grs, in_=grs), reads=[k("grs")], writes=[k("grs")])
            yield
            for h in range(4):
                self.ts(yn[:, h * 128:(h + 1) * 128], B[X[1]][:, h * 128:(h + 1) * 128], gmv[:, h, 0:1],
                        grs[:, h:h + 1], ALU.subtract, ALU.mult, reads=[bk(1), k("gmv"), k("grs")], writes=[k("yn")])
            yield
            self.tt(ya, yn, sg, ALU.mult, reads=[k("yn"), k("sg")], writes=[k("ya")])
            yield
            psY = Bb[X[0]].rearrange("p (a b) -> p a b", a=8)
            for k_ in range(4):
                self.tr(psY[:, k_, :], ya[:, k_ * 128:(k_ + 1) * 128], self.identb, reads=[k("ya")], writes=[bk(0)], inc=(k_ == 3))
            yield
            self.actf(YT[:, 0:4, tsl], psY[:, 0:4, :], AF.Copy, reads=[bk(0)], writes=[("YT", i)])

        self.interleave(p1gen, NT, width=2, lag=8)
        self.barrier()
        if self.stop_after == ("P1", l):
            self.tap("YT", YT, [128, 12, S])
            return

        self.off = P_OFF
        wB = self.Vat(self.SLOT[1], [128, KC, 1544], BF16)
        wC = self.Vat(self.SLOT[0], [128, KC, 1024], BF16)
        self.dma(pool, out=wC, in_=w_in[:, :, 3080:4104], writes=["slot0"], sem="slot0")
        foxb = self.A([128, 8], F32)
        self.dma(sp, out=foxb, in_=self.fox_b[l][0:1, :].partition_broadcast(128), writes=["foxb"], sem="small")
        KT = self.A([128, 4, S], BF16)
        KBc = self.A([128, S], BF16)
        Vaug = self.A([128, NT, 8, 65], BF16)
        q_bf = self.A([128, 512], BF16)
        k_bf = self.A([128, 512], BF16)
        z = self.A([128, 8], F32)
        ez = self.A([128, 8], F32)
        lf = self.A([128, 8], F32)
        negc = self.A([128, 8], F32)
        carry = self.A([128, 8], F32)
        r1 = self.A([128, 8], F32)
        r2 = self.A([128, 8], F32)
        BK = self.A([128, 128], BF16)
        BQ = self.A([128, 128], BF16)
        qTm8s = [self.A([128, 8, 128], BF16) for _ in range(2)]
        QBms = [self.A([128, 8, 128], BF16) for _ in range(2)]
        PT = [self.A([128, NT, 128], BF16) for _ in range(2)]
        rs = self.A([128, 8], F32)
        yb = self.A([128, 8, 64], BF16)
        assert self.off <= self.SLOT[0], ("P2 locals overlap weight slots", self.off, self.SLOT)
        self.op(dve, lambda: nc.vector.memset(carry, 0.0), writes=["carry"])
        self.op(dve, lambda: nc.vector.memset(BK, 0.0), writes=["BK"])
        self.op(dve, lambda: nc.vector.memset(BQ, 0.0), writes=["BQ"])
        self.op(dve, lambda: nc.vector.memset(Vaug.rearrange("p a b c -> p (a b c)"), 1.0), writes=["Vaug"])
        BK3 = BK[:, 0:48].rearrange("p (h r) -> p h r", h=8)
        BQ3 = BQ[:, 0:48].rearrange("p (h r) -> p h r", h=8)
        self.op(dve, lambda: nc.vector.memset(BK3[:, :, 3:6], 1.0), reads=[], writes=["BK"])
        self.op(dve, lambda: nc.vector.memset(BQ3[:, :, 0:3], 1.0), reads=[], writes=["BQ"])
        psT1 = Bb[2].rearrange("p (a b) -> p a b", a=8)
        psT2 = Bb[3].rearrange("p (a b) -> p a b", a=8)
        st8 = {"chunk": 0}

        def chain_stages(i):
            tsl = slice(i * 128, (i + 1) * 128)
            par = i % 2
            qTm8, QBm = qTm8s[par], QBms[par]
            qk_, qb_ = f"qTm8_{par}", f"QBm_{par}"

            def c1():
                for kc in range(KC):
                    self.mm(B[0], xT[:, kc, tsl], wB[:, kc, 0:512], kc == 0, kc == KC - 1, reads=["slot1"], writes=["B0"])
                for kc in range(KC):
                    self.mm(B[1], xT[:, kc, tsl], wB[:, kc, 512:1024], kc == 0, kc == KC - 1, reads=["slot1"], writes=["B1"])

            def c2():
                self.actf(q_bf, B[0], AF.Copy, reads=["B0"], writes=["q_bf"])
                self.actf(k_bf, B[1], AF.Identity, reads=["B1"], writes=["k_bf"], scale=0.125)

            def c3():
                for kc in range(KC):
                    self.mm(B[0], xT[:, kc, tsl], wB[:, kc, 1024:1536], kc == 0, kc == KC - 1, reads=["slot1"], writes=["B0"])
                for kc in range(KC):
                    self.mm(B[1][:, 0:8], xT[:, kc, tsl], wB[:, kc, 1536:1544], kc == 0, kc == KC - 1, reads=["slot1"], writes=["B1"])
                for k_ in range(4):
                    self.tr(psT1[:, k_, :], q_bf[:, k_ * 128:(k_ + 1) * 128], self.identb, reads=["q_bf", "cb"], writes=["B2"], inc=False)
                for k_ in range(4):
                    self.tr(psT1[:, 4 + k_, :], k_bf[:, k_ * 128:(k_ + 1) * 128], self.identb, reads=["k_bf"], writes=["B2"], inc=(k_ == 3))

            def c4():
                self.actf(Vaug[:, i, :, 0:64], B[0].rearrange("p (h d) -> p h d", h=8), AF.Copy, reads=["B0"], writes=[("V", i)])
                self.tt(z, B[1][:, 0:8], foxb, ALU.add, reads=["B1", "foxb"], writes=["z"])
                self.actf(ez, z, AF.Exp, reads=["z"], writes=["ez"], scale=-1.0)
                self.actf(lf, ez, AF.Ln, reads=["ez"], writes=["lf"], bias=1.0, scale=1.0)
                qTm84 = qTm8.rearrange("p (a b) t -> p a b t", a=4)
                for pr_ in range(2):
                    self.ts(qTm84[:, :, pr_, :], psT1[:, 0:4, :], self.pmask[:, pr_:pr_ + 1], None, ALU.mult,
                            reads=["B2"], writes=[qk_])
                self.actf(KT[:, :, tsl], psT1[:, 4:8, :], AF.Copy, reads=["B2"], writes=[("KT", i)])

            def c5():
                self.mm(B[1][:, 8:16], self.tri, lf, True, True, reads=["lf", "cf"], writes=["B1"], inc=False)
                self.mm(B[1][:, 16:24], self.onesf, lf, True, True, reads=["lf"], writes=["B1"], inc=True)

            def c6():
                self.tt(negc, B[1][:, 8:16], carry, ALU.add, reads=["B1", "carry"], writes=["negc"])
                self.tt(carry, B[1][:, 16:24], carry, ALU.add, reads=["B1", "carry"], writes=["carry"])
                self.vcopy(BK3[:, :, 0], negc, reads=["negc"], writes=["BKa"])
                self.tt(r1, negc, BK3[:, :, 0], ALU.subtract, reads=["negc", "BKa"], writes=["r1"])
                self.vcopy(BK3[:, :, 1], r1, reads=["r1"], writes=["BKb"])
                self.tt(r2, r1, BK3[:, :, 1], ALU.subtract, reads=["r1", "BKb"], writes=["r2"])
                self.vcopy(BK3[:, :, 2], r2, reads=["r2"], writes=["BKc"])
                self.ts(BQ3[:, :, 3:6], BK3[:, :, 0:3], -1.0, None, ALU.mult, reads=["BKa", "BKb", "BKc"], writes=["BQ"])

            def c7():
                self.tr(psT2[:, 0, :], BK, self.identb, reads=["BK", "BKa", "BKb", "BKc"], writes=["B3"], inc=False)
                self.tr(psT2[:, 1, :], BQ, self.identb, reads=["BQ"], writes=["B3"], inc=True)

            def c8():
                self.actf(KBc[:, tsl], psT2[:, 0, :], AF.Copy, reads=["B3"], writes=[("KB", i)])
                self.tt(QBm, psT2[:, 1, :].unsqueeze(1).broadcast_to([128, 8, 128]), self.hmask, ALU.mult,
                        reads=["B3", "cb"], writes=[qb_])
            return [c1, c2, c3, c4, c5, c6, c7, c8]

        def head(i, h):
            par = i % 2
            qTm8, QBm = qTm8s[par], QBms[par]
            qk_, qb_ = f"qTm8_{par}", f"QBm_{par}"
            PTb = PT[h % 2]
            pk = f"PT{h % 2}"
            for c0 in range(0, i + 1, 4):
                nb = min(4, i + 1 - c0)
                bk = 4 + (st8["chunk"] % 2)
                st8["chunk"] += 1
                for jj in range(nb):
                    j = c0 + jj
                    self.mm(B[bk][:, jj * 128:(jj + 1) * 128], KT[:, h // 2, j * 128:(j + 1) * 128], qTm8[:, h, :],
                            True, False, reads=[("KT", j), qk_], writes=[f"B{bk}"], inc=False)
                    self.mm(B[bk][:, jj * 128:(jj + 1) * 128], KBc[:, j * 128:(j + 1) * 128], QBm[:, h, :],
                            False, True, reads=[("KB", j), qb_], writes=[f"B{bk}"], inc=(jj == nb - 1))
                if c0 + nb == i + 1:
                    jj = nb - 1
                    self.tt(B[bk][:, jj * 128:(jj + 1) * 128], B[bk][:, jj * 128:(jj + 1) * 128], self.negm, ALU.add,
                            reads=[f"B{bk}"], writes=[f"B{bk}"])
                self.actf(PTb[:, c0:c0 + nb, :].rearrange("p a b -> p (a b)"), B[bk][:, 0:nb * 128], AF.Exp,
                          reads=[f"B{bk}"], writes=[pk])
            ob = 6 + h // 4
            for j in range(i + 1):
                self.mm(B[ob][:, (h % 4) * 65:(h % 4) * 65 + 65], PTb[:, j, :], Vaug[:, j, h, :], j == 0, j == i,
                        reads=[pk, ("V", j), "Vaug"], writes=[f"B{ob}"], inc=(j == i))

        def finish(i):
            tsl = slice(i * 128, (i + 1) * 128)
            for hb in range(2):
                po = B[6 + hb][:, 0:260].rearrange("p (h c) -> p h c", h=4)
                self.op(dve, lambda po=po, hb=hb: nc.vector.reciprocal(out=rs[:, hb * 4:(hb + 1) * 4], in_=po[:, :, 64]),
                        reads=[f"B{6 + hb}"], writes=["rs"])
                self.tt(yb[:, hb * 4:(hb + 1) * 4, :], po[:, :, 0:64],
                        rs[:, hb * 4:(hb + 1) * 4].unsqueeze(2).broadcast_to([128, 4, 64]), ALU.mult,
                        reads=[f"B{6 + hb}", "rs"], writes=["yb"])
            ybf = yb.rearrange("p a b -> p (a b)")
            for k_ in range(4):
                self.tr(psT2[:, 2 + k_, :], ybf[:, k_ * 128:(k_ + 1) * 128], self.identb, reads=["yb"], writes=["B3"], inc=(k_ == 3))
            self.actf(YT[:, 4:8, tsl], psT2[:, 2:6, :], AF.Copy, reads=["B3"], writes=[("YT", i)])

        for c_ in chain_stages(0):
            c_()
        for i in range(NT):
            nxt = chain_stages(i + 1) if i + 1 < NT else []
            for h in range(8):
                if h < len(nxt):
                    nxt[h]()
                head(i, h)
            finish(i)
        self.barrier()
        if self.stop_after == ("P2", l):
            self.tap("YT", YT, [128, 12, S])
            return

        self.off = P_OFF
        wO = self.Vat(self.SLOT[1], [128, KC, D], BF16)
        self.dma(pool, out=wO, in_=self.w_out[l].rearrange("(kc p) n -> p kc n", p=128), writes=["slot1"], sem="slot1")
        wsf = self.A([128, 512], F32)
        WST = self.A([128, 4, 128], BF16)
        bsT = self.A([128, 4], F32)
        gmg = self.A([128, 512], F32)
        gmb = self.A([128, 512], F32)
        self.dma(sp, out=wsf, in_=self.gm_wsT[l][:, :], writes=["wsf"], sem="small")
        self.dma(sp, out=bsT, in_=self.gm_bsT[l][:, :], writes=["bsT"], sem="small")
        self.dma(sp, out=gmg, in_=self.gm_ln[l][0:1, :].partition_broadcast(128), writes=["gmg"], sem="small")
        self.dma(sp, out=gmb, in_=self.gm_ln[l][1:2, :].partition_broadcast(128), writes=["gmb"], sem="small")
        for r_ in ("wsf", "bsT", "gmg", "gmb", "foxb"):
            self.R(r_).w = (self.dsem("small"), self.dsem("small").cnt)
        self.tt(WST, wsf.rearrange("p (a b) -> p a b", a=4), self.mask01.unsqueeze(1).broadcast_to([128, 4, 128]),
                ALU.mult, reads=["wsf", "cb"], writes=["WST"])
        us = [self.A([128, 512], F32) for _ in range(2)]
        gvs = [self.A([128, 512], F32) for _ in range(2)]
        vbs = [self.A([128, 512], BF16) for _ in range(2)]
        ycs = [self.A([128, 512], BF16) for _ in range(2)]

        def p3gen(i):
            tsl = slice(i * 128, (i + 1) * 128)
            par = i % 2
            X = [4 * par + j for j in range(4)]
            bk = lambda j: f"B{X[j]}"
            u, gvv, vb, yc = us[par], gvs[par], vbs[par], ycs[par]
            gk, ks = f"gvv{par}", f"_{par}"
            for c in range(2):
                for kc in range(KC):
                    self.mm(B[X[c]], xT[:, kc, tsl], wC[:, kc, c * 512:(c + 1) * 512], kc == 0, kc == KC - 1,
                            reads=["slot0"], writes=[bk(c)])
            yield
            self.actf(gvv, B[X[1]], AF.Gelu_apprx_tanh, reads=[bk(1)], writes=[gk])
            self.actf(u, B[X[0]], AF.Gelu_apprx_tanh, reads=[bk(0)], writes=[f"u{par}"])
            yield
            st_, mv_, rs_ = self.lnt[par]
            self.op(dve, lambda: nc.vector.bn_stats(out=st_[:, 0, :], in_=gvv), reads=[gk], writes=["ln_st" + ks])
            yield
            self.op(dve, lambda: nc.vector.bn_aggr(out=mv_, in_=st_[:, 0, :]), reads=["ln_st" + ks], writes=["ln_mv" + ks])
            yield
            self.actf(rs_, mv_[:, 1:2], AF.Sqrt, reads=["ln_mv" + ks], writes=["ln_rstd" + ks], bias=LN_EPS, scale=1.0)
            yield
            self.op(dve, lambda: nc.vector.reciprocal(out=rs_, in_=rs_), reads=["ln_rstd" + ks], writes=["ln_rstd" + ks])
            yield
            self.ts(gvv, gvv, mv_[:, 0:1], rs_[:, 0:1], ALU.subtract, ALU.mult, reads=[gk, "ln_mv" + ks, "ln_rstd" + ks], writes=[gk])
            yield
            self.tt(gvv, gvv, gmg, ALU.mult, reads=[gk, "gmg"], writes=[gk])
            yield
            self.tt(vb, gvv, gmb, ALU.add, reads=[gk, "gmb"], writes=[f"vb{par}"])
            yield
            for g in range(4):
                self.mm(B[X[2]][:, g * 128:(g + 1) * 128], WST[:, g, :], vb[:, g * 128:(g + 1) * 128], True, True,
                        reads=["WST", f"vb{par}"], writes=[bk(2)], inc=(g == 3))
            yield
            for g in range(4):
                self.stt(yc[:, g * 128:(g + 1) * 128], B[X[2]][:, g * 128:(g + 1) * 128], bsT[:, g:g + 1],
                         u[:, g * 128:(g + 1) * 128], ALU.add, ALU.mult, reads=[bk(2), "bsT", f"u{par}"], writes=[f"yc{par}"])
            yield
            psT = Bb[X[3]].rearrange("p (a b) -> p a b", a=8)
            for k_ in range(4):
                self.tr(psT[:, k_, :], yc[:, k_ * 128:(k_ + 1) * 128], self.identb, reads=[f"yc{par}"], writes=[bk(3)], inc=(k_ == 3))
            yield
            self.actf(YT[:, 8:12, tsl], psT[:, 0:4, :], AF.Copy, reads=[bk(3)], writes=[("YT", i)])

        self.interleave(p3gen, NT, width=2, lag=5)
        self.barrier()
        if self.stop_after == ("P3", l):
            self.tap("YT", YT, [128, 12, S])
            return

        self.off = self.MBASE + 65536
        mergedT = self.A([128, KC, S], BF16)
        P5_OFF = self.off
        gbT = self.A([128, 24], F32)
        self.dma(sp, out=gbT, in_=self.gate_bT[l][:, :], writes=["gbT"], sem="small")
        wG = [self.A([128, KC, 3, 128], BF16) for _ in range(2)]
        wBr = [self.A([128, 4, 3, 128], BF16) for _ in range(2)]
        sgt = [self.A([128, 512], F32) for _ in range(2)]
        macc = self.A([128, 512], F32)
        mtmp = self.A([128, 512], F32)
        w_br = self.w_branch[l]
        assert self.off <= self.SLOT[1], ("P4 locals overlap wO slot", self.off, self.SLOT)

        def load_g(dc):
            s_ = dc % 2
            for n in range(3):
                c0 = 4104 + n * 1024 + dc * 128
                self.dma(pool, out=wG[s_][:, :, n, :], in_=w_in[:, :, c0:c0 + 128], writes=[f"wG{s_}"], sem=f"wG{s_}")
                self.dma(pool, out=wBr[s_][:, :, n, :],
                         in_=w_br[n].rearrange("(kc p) d -> p kc d", p=128)[:, :, dc * 128:(dc + 1) * 128],
                         writes=[f"wG{s_}"], sem=f"wG{s_}")
        load_g(0)
        cnt = 0
        for dc in range(KC):
            if dc + 1 < KC:
                load_g(dc + 1)
            s_ = dc % 2
            for g in range(4):
                gsl = slice(g * 512, (g + 1) * 512)
                for n in range(3):
                    pg, pb = cnt % 2, 2 + cnt % 2
                    sb_ = sgt[cnt % 2]
                    sk = f"sg{cnt % 2}"
                    cnt += 1
                    for kc in range(KC):
                        self.mm(B[pg], wG[s_][:, kc, n, :], xT[:, kc, gsl], kc == 0, kc == KC - 1, reads=[f"wG{s_}"], writes=[f"B{pg}"])
                    for kc in range(4):
                        self.mm(B[pb], wBr[s_][:, kc, n, :], YT[:, n * 4 + kc, gsl], kc == 0, kc == 3, reads=[f"wG{s_}"], writes=[f"B{pb}"])
                    self.actf(sb_, B[pg], AF.Sigmoid, reads=[f"B{pg}", "gbT"], writes=[sk],
                              bias=gbT[:, n * 8 + dc:n * 8 + dc + 1], scale=1.0)
                    if n == 0:
                        self.tt(macc, sb_, B[pb], ALU.mult, reads=[sk, f"B{pb}"], writes=["macc"])
                    elif n == 1:
                        self.tt(mtmp, sb_, B[pb], ALU.mult, reads=[sk, f"B{pb}"], writes=["mtmp"])
                        self.tt(macc, macc, mtmp, ALU.add, reads=["macc", "mtmp"], writes=["macc"])
                    else:
                        self.tt(mtmp, sb_, B[pb], ALU.mult, reads=[sk, f"B{pb}"], writes=["mtmp"])
                        self.tt(mergedT[:, dc, gsl], macc, mtmp, ALU.add, reads=["macc", "mtmp"], writes=[("mT", dc, g)])
        self.barrier()
        if self.stop_after == ("P4", l):
            self.tap("mergedT", mergedT, [128, KC, S])
            return

        self.off = self.MBASE
        acc = self.acc = self.A([128, NT, D], F32)
        self.off = P5_OFF
        moe = (l % 2 == 1)
        self.dma(sp, out=self.lng, in_=self.ln_gb[l][0:1, :].partition_broadcast(128), writes=["lng"], sem="lnp")
        self.dma(sp, out=self.lnb, in_=self.ln_gb[l][1:2, :].partition_broadcast(128), writes=["lnb"], sem="lnp")
        for r_ in ("lng", "lnb"):
            self.R(r_).w = (self.dsem("lnp"), self.dsem("lnp").cnt)
        xr = [self.A([128, D], F32) for _ in range(2)]
        sress = [self.A([128, D], F32) for _ in range(2)]
        xbf = self.A([128, D], BF16)
        xsrc = self.x if l == 0 else self.xres
        if moe:
            wR = self.A([128, KC, NEXP], F32)
            self.dma(sp, out=wR, in_=self.router.rearrange("(kc p) e -> p kc e", p=128), writes=["wR"], sem="small")
            x1T = self.A([128, KC, 128], F32)
            lgt = self.A([128, 8], F32)
            mx8 = self.A([128, 8], F32)
            nm1 = self.A([128, 1], F32)
            msk = self.A([128, 8], F32)
            eg = self.A([128, 8], F32)
            ssum = self.A([128, 1], F32)

        xbfs = [xbf, self.A([128, D], BF16)]
        assert self.off <= self.SLOT[1], ("P5 locals overlap wO slot", self.off, self.SLOT)

        def p5gen(i):
            tsl = slice(i * 128, (i + 1) * 128)
            b = i % 2
            sres = sress[b]
            sk_, ks = f"sres{b}", f"_{b}"
            xb_ = xbfs[b]
            st_, mv_, rs_ = self.lnt[b]
            self.dma(sp, out=xr[b], in_=xsrc[tsl, :], reads=[("xres", i)], writes=[f"xr{b}"], sem=f"xr{b}")
            for hf in range(2):
                pb_ = 2 * b + hf
                for kc in range(KC):
                    self.mm(B[pb_], mergedT[:, kc, tsl], wO[:, kc, hf * 512:(hf + 1) * 512], kc == 0, kc == KC - 1,
                            reads=["slot1"], writes=[f"B{pb_}"])
            yield
            for hf in range(2):
                pb_ = 2 * b + hf
                self.stt(sres[:, hf * 512:(hf + 1) * 512], xr[b][:, hf * 512:(hf + 1) * 512], float(ALPHA), B[pb_],
                         ALU.mult, ALU.add, reads=[f"xr{b}", f"B{pb_}"], writes=[sk_])
                yield
            for c in range(2):
                self.op(dve, lambda c=c: nc.vector.bn_stats(out=st_[:, c, :], in_=sres[:, c * 512:(c + 1) * 512]),
                        reads=[sk_], writes=["ln_st" + ks])
            yield
            self.op(dve, lambda: nc.vector.bn_aggr(out=mv_, in_=st_.rearrange("p a b -> p (a b)")), reads=["ln_st" + ks], writes=["ln_mv" + ks])
            yield
            self.actf(rs_, mv_[:, 1:2], AF.Sqrt, reads=["ln_mv" + ks], writes=["ln_rstd" + ks], bias=LN_EPS, scale=1.0)
            yield
            self.op(dve, lambda: nc.vector.reciprocal(out=rs_, in_=rs_), reads=["ln_rstd" + ks], writes=["ln_rstd" + ks])
            yield
            self.ts(sres, sres, mv_[:, 0:1], rs_[:, 0:1], ALU.subtract, ALU.mult, reads=[sk_, "ln_mv" + ks, "ln_rstd" + ks], writes=[sk_])
            yield
            self.tt(sres, sres, self.lng, ALU.mult, reads=[sk_, "lng"], writes=[sk_])
            yield
            self.tt(sres, sres, self.lnb, ALU.add, reads=[sk_, "lnb"], writes=[sk_])
            yield
            self.actf(xb_, sres, AF.Copy, reads=[sk_], writes=[f"xbf{b}"])
            self.actf(acc[:, i, :], sres, AF.Identity, reads=[sk_], writes=[("acc", i)], scale=float(ALPHA))
            yield
            psT = Bb[4 + b].rearrange("p (a b) -> p a b", a=8)
            for kc in range(KC):
                self.tr(psT[:, kc, :], xb_[:, kc * 128:(kc + 1) * 128], self.identb, reads=[f"xbf{b}"], writes=[f"B{4 + b}"], inc=(kc == KC - 1))
            yield
            self.actf(xT[:, :, tsl], psT, AF.Copy, reads=[f"B{4 + b}"], writes=[("xT", i)])
            yield
            if moe:
                for kc in range(KC):
                    bk = 6 + kc // 4
                    self.tr(B[bk][:, (kc % 4) * 128:(kc % 4 + 1) * 128], sres[:, kc * 128:(kc + 1) * 128], self.identf,
                            reads=[sk_, "cf"], writes=[f"B{bk}"], inc=(kc % 4 == 3))
                yield
                for hb in range(2):
                    self.vcopy(x1T[:, hb * 4:(hb + 1) * 4, :].rearrange("p a b -> p (a b)"), B[6 + hb],
                               reads=[f"B{6 + hb}"], writes=["x1T"])
                yield
                for kc in range(KC):
                    self.mm(B[6][:, 0:8], x1T[:, kc, :], wR[:, kc, :], kc == 0, kc == KC - 1, reads=["x1T", "wR"], writes=["B6"])
                yield
                self.vcopy(lgt, B[6][:, 0:8], reads=["B6"], writes=["lgt"])
                self.op(dve, lambda: nc.vector.max(out=mx8, in_=lgt), reads=["lgt"], writes=["mx8"])
                self.ts(nm1, mx8[:, 0:1], -1.0, None, ALU.mult, reads=["mx8"], writes=["nm1"])
                self.ts(msk, lgt, mx8[:, 1:2], None, ALU.is_ge, reads=["lgt", "mx8"], writes=["msk"])
                self.actf(eg, lgt, AF.Exp, reads=["lgt", "nm1"], writes=["eg"], bias=nm1[:, 0:1], scale=1.0)
                self.tt(eg, eg, msk, ALU.mult, reads=["eg", "msk"], writes=["eg"])
                self.op(dve, lambda: nc.vector.reduce_sum(out=ssum, in_=eg, axis=mybir.AxisListType.X), reads=["eg"], writes=["ssum"])
                self.op(dve, lambda: nc.vector.reciprocal(out=ssum, in_=ssum), reads=["ssum"], writes=["ssum"])
                self.ts(self.gates[:, i, :], eg, ssum[:, 0:1], None, ALU.mult, reads=["eg", "ssum"], writes=[("gates", i)])

        self.interleave(p5gen, NT, width=2, lag=5)
        self.barrier()
        if self.stop_after == ("P5", l):
            self.tap("acc", acc, [128, NT, D])
            self.tap("gates", self.gates, [128, NT, NEXP])
            return

    def ffn(self, l):
        nc = self.nc
        pe, act, dve, pool, sp = self.pe, self.act, self.dve, self.pool, self.sp
        xT = self.xT
        moe = (l % 2 == 1)
        last = (l == self.n_layers - 1)
        B = [self.bank(b) for b in range(8)]
        Bb = [self.bank(b, BF16) for b in range(8)]
        self.off = self.MBASE
        acc = self.A([128, NT, D], F32)
        hT = self.A([128, 4, S], BF16)
        upb = [self.A([128, KC, 2, 512], BF16) for _ in range(2)]
        dnb = [self.A([128, 4, D], BF16) for _ in range(2)]
        sa = [self.A([128, 512], F32) for _ in range(2)]
        if moe:
            F = FF_EXP
            segs = [(e, f0, 4) for e in range(getattr(self, 'dbg_nexp', NEXP)) for f0 in range(0, F // 128, 4)]
        else:
            F = FF_DENSE
            nch = F // 128
            segs = [(0, f0, min(4, nch - f0)) for f0 in range(0, nch, 4)]

        def wup(e):
            t = self.moe_up[e] if moe else self.dense_up
            return t.rearrange("(kc p) n -> p kc n", p=128)

        def wdn(e):
            t = self.moe_down[e] if moe else self.dense_down
            return t.rearrange("(fc p) d -> p fc d", p=128)

        def load(q):
            e, f0, nch = segs[q]
            s_ = q % 2
            w = nch * 128
            self.dma(pool, out=upb[s_][:, :, 0, 0:w], in_=wup(e)[:, :, f0 * 128:f0 * 128 + w], writes=[f"up{s_}"], sem=f"up{s_}")
            self.dma(pool, out=upb[s_][:, :, 1, 0:w], in_=wup(e)[:, :, F + f0 * 128:F + f0 * 128 + w], writes=[f"up{s_}"], sem=f"up{s_}")
            self.dma(pool, out=dnb[s_][:, 0:nch, :], in_=wdn(e)[:, f0:f0 + nch, :], writes=[f"dn{s_}"], sem=f"dn{s_}")

        load(0)
        ucnt = 0
        ocnt = 0
        for q, (e, f0, nch) in enumerate(segs):
            if q + 1 < len(segs):
                load(q + 1)
            s_ = q % 2
            for fc in range(nch):
                for g in range(4):
                    gsl = slice(g * 512, (g + 1) * 512)
                    pa, pb = ucnt % 2, 2 + ucnt % 2
                    sab = sa[ucnt % 2]
                    sk = f"sa{ucnt % 2}"
                    ucnt += 1
                    for kc in range(KC):
                        self.mm(B[pa], upb[s_][:, kc, 0, fc * 128:(fc + 1) * 128], xT[:, kc, gsl], kc == 0, kc == KC - 1,
                                reads=[f"up{s_}"], writes=[f"B{pa}"])
                    for kc in range(KC):
                        self.mm(B[pb], upb[s_][:, kc, 1, fc * 128:(fc + 1) * 128], xT[:, kc, gsl], kc == 0, kc == KC - 1,
                                reads=[f"up{s_}"], writes=[f"B{pb}"])
                    self.actf(sab, B[pa], AF.Silu, reads=[f"B{pa}"], writes=[sk])
                    self.tt(hT[:, fc, gsl], sab, B[pb], ALU.mult, reads=[sk, f"B{pb}"], writes=[("hT", fc, g)])
            for i in range(NT):
                tsl = slice(i * 128, (i + 1) * 128)
                for hf in range(2):
                    po = 4 + ocnt % 4
                    ocnt += 1
                    for fc in range(nch):
                        self.mm(B[po], hT[:, fc, tsl], dnb[s_][:, fc, hf * 512:(hf + 1) * 512], fc == 0, fc == nch - 1,
                                reads=[("hT", fc, i // 4), f"dn{s_}"], writes=[f"B{po}"])
                    asl = acc[:, i, hf * 512:(hf + 1) * 512]
                    if moe:
                        self.stt(asl, B[po], self.gates[:, i, e:e + 1], asl, ALU.mult, ALU.add,
                                 reads=[f"B{po}", ("acc", i, hf)], writes=[("acc", i, hf)])
                    else:
                        self.tt(asl, B[po], asl, ALU.add, reads=[f"B{po}", ("acc", i, hf)], writes=[("acc", i, hf)])
        self.barrier()
        if self.stop_after == ("F1", l):
            self.tap("acc", acc, [128, NT, D])
            return
        self.off = self.MBASE + 65536
        self.dma(sp, out=self.lng, in_=self.ln_gb[l][2:3, :].partition_broadcast(128), writes=["lng"], sem="lnp")
        self.dma(sp, out=self.lnb, in_=self.ln_gb[l][3:4, :].partition_broadcast(128), writes=["lnb"], sem="lnp")
        for r_ in ("lng", "lnb"):
            self.R(r_).w = (self.dsem("lnp"), self.dsem("lnp").cnt)
        xo = [self.A([128, D], F32) for _ in range(2)]
        xbfs = [self.A([128, D], BF16) for _ in range(2)]
        dst = self.y if last else self.xres
        if not last:
            self.prefetch_AB(l + 1)

        def lngen(i):
            tsl = slice(i * 128, (i + 1) * 128)
            b = i % 2
            ks = f"_{b}"
            st_, mv_, rs_ = self.lnt[b]
            ak = [("acc", i, 0), ("acc", i, 1)]
            for c in range(2):
                self.op(dve, lambda c=c: nc.vector.bn_stats(out=st_[:, c, :], in_=acc[:, i, c * 512:(c + 1) * 512]),
                        reads=ak, writes=["ln_st" + ks])
            yield
            self.op(dve, lambda: nc.vector.bn_aggr(out=mv_, in_=st_.rearrange("p a b -> p (a b)")), reads=["ln_st" + ks], writes=["ln_mv" + ks])
            yield
            self.actf(rs_, mv_[:, 1:2], AF.Sqrt, reads=["ln_mv" + ks], writes=["ln_rstd" + ks], bias=LN_EPS, scale=1.0)
            yield
            self.op(dve, lambda: nc.vector.reciprocal(out=rs_, in_=rs_), reads=["ln_rstd" + ks], writes=["ln_rstd" + ks])
            yield
            self.ts(xo[b], acc[:, i, :], mv_[:, 0:1], rs_[:, 0:1], ALU.subtract, ALU.mult,
                    reads=ak + ["ln_mv" + ks, "ln_rstd" + ks], writes=[f"xo{b}"])
            yield
            self.tt(xo[b], xo[b], self.lng, ALU.mult, reads=[f"xo{b}", "lng"], writes=[f"xo{b}"])
            yield
            self.tt(xo[b], xo[b], self.lnb, ALU.add, reads=[f"xo{b}", "lnb"], writes=[f"xo{b}"])
            yield
            self.dma(sp, out=dst[tsl, :], in_=xo[b], reads=[f"xo{b}"], writes=[("xres", i)], sem=f"xo{b}")
            if not last:
                self.actf(xbfs[b], xo[b], AF.Copy, reads=[f"xo{b}"], writes=[f"xbf{b}"])
                yield
                psT = Bb[b].rearrange("p (a b) -> p a b", a=8)
                for kc in range(KC):
                    self.tr(psT[:, kc, :], xbfs[b][:, kc * 128:(kc + 1) * 128], self.identb, reads=[f"xbf{b}"], writes=[f"B{b}"],
                            inc=(kc == KC - 1))
                yield
                self.actf(xT[:, :, tsl], psT, AF.Copy, reads=[f"B{b}"], writes=[("xT", i)])

        self.interleave(lngen, NT, width=2, lag=4)
        self.barrier()


def prep_inputs(inputs, n_layers=2):
    f = lambda a: np.ascontiguousarray(np.asarray(a, dtype=np.float32))
    cf, cb = host_consts()
    shared = {"constf": cf, "constb": cb}
    for l in range(n_layers):
        shared[f"w_in{l}"] = f(inputs["w_in"][l])
        shared[f"fox_b{l}"] = f(np.asarray(inputs["fox_b_f"])[l].reshape(1, 8))
        shared[f"gate_bT{l}"] = f(np.asarray(inputs["gate_b"])[l].reshape(24, 128).T)
        shared[f"gm_wsT{l}"] = f(np.asarray(inputs["gm_w_s"])[l].transpose(2, 0, 1).reshape(128, 512))
        shared[f"gm_bsT{l}"] = f(np.asarray(inputs["gm_b_s"])[l].T)
        shared[f"gm_ln{l}"] = f(np.stack([np.asarray(inputs["gm_ln_g"])[l], np.asarray(inputs["gm_ln_b"])[l]]))
        shared[f"w_branch{l}"] = f(inputs["w_branch"][l])
        shared[f"w_out{l}"] = f(inputs["w_out"][l])
        lg, lb = np.asarray(inputs["ln_g"])[l], np.asarray(inputs["ln_b"])[l]
        shared[f"ln_gb{l}"] = f(np.stack([lg[0], lb[0], lg[1], lb[1]]))
    shared["dense_up"] = f(inputs["dense_w_up"][0])
    shared["dense_down"] = f(inputs["dense_w_down"][0])
    if n_layers > 1:
        shared["router"] = f(inputs["moe_router"][0])
        shared["moe_up"] = f(inputs["moe_w_up"][0])
        shared["moe_down"] = f(inputs["moe_w_down"][0])
    return shared


def kernel(**inputs):
    x = np.asarray(inputs["x"], dtype=np.float32)
    nb = x.shape[0]
    kb = KB(n_layers=DEPTH)
    nc = kb.build()
    shared = prep_inputs(inputs, DEPTH)
    in_maps = []
    for b in range(nb):
        m = dict(shared)
        m["x"] = np.ascontiguousarray(x[b])
        in_maps.append(m)
    res = run_bass_kernel_spmd(nc, in_maps, core_ids=list(range(nb)))
    out = np.stack([np.asarray(res.results[b]["y"], dtype=np.float32) for b in range(nb)], axis=0)
    return out
```

```python
import numpy as np
import concourse.bass as bass
import concourse.mybir as mybir
from concourse.bass_utils import run_bass_kernel_spmd

F32 = mybir.dt.float32
BF16 = mybir.dt.bfloat16
U8 = mybir.dt.uint8
AF = mybir.ActivationFunctionType
ALU = mybir.AluOpType

S = 2048
D = 1024
NT = 16
KC = 8
N_IN = 7176
DEPTH = 2
FF_DENSE = 2816
FF_EXP = 3584
NEXP = 8
ALPHA = (2 * DEPTH) ** 0.25
LN_EPS = 1e-5
GN_EPS = 1e-6
NEG = -30000.0

CF_COS, CF_SIN, CF_DECIN, CF_KDM, CF_QDEC, CF_SMALL, CF_TRI, CF_IDENT, CF_NEGM, CF_ONES = (
    0, 512, 1024, 1536, 2048, 2304, 2308, 2436, 2564, 2692)
NCF1 = 2304
NCF = 2820
CB_HMASK, CB_MASK, CB_IDENT = 0, 1024, 1152
NCB = 1280


def host_consts():
    lg = np.log1p(-np.exp2(-5.0 - np.arange(4, dtype=np.float64)))
    p = np.arange(128)
    cf = np.zeros((128, NCF), np.float32)
    pos = np.arange(S, dtype=np.float32)
    inv_freq = (10000.0 ** (-np.arange(32, dtype=np.float32) / 32)).astype(np.float32)
    ang = (pos[:, None] * inv_freq[None, :]).astype(np.float32)
    cos = np.cos(ang).astype(np.float32).reshape(NT, 128, 32).transpose(1, 0, 2).reshape(128, 512)
    sin = np.sin(ang).astype(np.float32).reshape(NT, 128, 32).transpose(1, 0, 2).reshape(128, 512)
    cf[:, CF_COS:CF_COS + 512] = cos
    cf[:, CF_SIN:CF_SIN + 512] = sin
    j = p[:, None]
    i = p[None, :]
    decin = np.zeros((128, 4, 128))
    kdm = np.zeros((128, 4, 128))
    qdec = np.zeros((128, 4, 64))
    for h in range(4):
        decin[:, h, :] = np.where(i >= j, np.exp(lg[h] * np.maximum(i - j, 0)), 0.0) * 0.125
        hl = h % 2
        kdm[:, h, hl * 64:(hl + 1) * 64] = (np.exp(lg[h] * (127 - p)) * 0.125)[:, None]
        qdec[:, h, :] = np.exp(lg[h] * (p + 1.0))[:, None]
    cf[:, CF_DECIN:CF_DECIN + 512] = decin.reshape(128, 512)
    cf[:, CF_KDM:CF_KDM + 512] = kdm.reshape(128, 512)
    cf[:, CF_QDEC:CF_QDEC + 256] = qdec.reshape(128, 256)
    cf[:, CF_SMALL + 0] = (p < 64)
    cf[:, CF_SMALL + 1] = (p >= 64)
    for pr in range(2):
        cf[:, CF_SMALL + 2 + pr] = np.exp(np.where(p < 64, lg[2 * pr], lg[2 * pr + 1]) * 128.0)
    tri = (j <= i).astype(np.float32)
    cf[:, CF_TRI:CF_TRI + 128] = tri
    cf[:, CF_IDENT:CF_IDENT + 128] = np.eye(128, dtype=np.float32)
    cf[:, CF_NEGM:CF_NEGM + 128] = np.where(j <= i, 0.0, NEG)
    cf[:, CF_ONES:CF_ONES + 128] = 1.0
    cb = np.zeros((128, NCB), np.float32)
    hm = np.zeros((128, 8, 128), np.float32)
    for h in range(8):
        hm[h * 6:(h + 1) * 6, h, :] = 1.0
    cb[:, CB_HMASK:CB_HMASK + 1024] = hm.reshape(128, 1024)
    cb[:, CB_MASK:CB_MASK + 128] = tri
    cb[:, CB_IDENT:CB_IDENT + 128] = np.eye(128, dtype=np.float32)
    return cf, cb


class Eng:
    def __init__(self, nc, eng, name):
        self.eng = eng
        self.sem = nc.alloc_semaphore("s_" + name)
        self.cnt = 0
        self.seen = {}
        self.name = name


class DSem:
    def __init__(self, nc, name):
        self.sem = nc.alloc_semaphore("d_" + name)
        self.cnt = 0
        self.name = name


class Res:
    __slots__ = ("w", "r")

    def __init__(self):
        self.w = None
        self.r = {}


class Cut(Exception):
    pass


class KB:
    def ck(self, n):
        if self.cutn == n:
            raise Cut()

    def __init__(self, n_layers=2, debug=False, stop_after=None, cutn=None):
        self.cutn = cutn
        self.n_layers = n_layers
        self.debug = debug
        self.stop_after = stop_after
        self.taps = []
        nc = self.nc = bass.Bass("TRN2", target_bir_lowering=False)
        self.pe = Eng(nc, nc.tensor, "pe")
        self.act = Eng(nc, nc.scalar, "act")
        self.dve = Eng(nc, nc.vector, "dve")
        self.pool = Eng(nc, nc.gpsimd, "pool")
        self.sp = Eng(nc, nc.sync, "sp")
        self.engs = [self.pe, self.act, self.dve, self.pool, self.sp]
        self.dsems = {}
        self.pending = None
        self.res = {}
        self.inputs = {}
        di = self.din
        self.x = di("x", [S, D])
        self.constf = di("constf", [128, NCF])
        self.constb = di("constb", [128, NCB])
        L = n_layers
        self.w_in = [di(f"w_in{l}", [D, N_IN]) for l in range(L)]
        self.fox_b = [di(f"fox_b{l}", [1, 8]) for l in range(L)]
        self.gate_bT = [di(f"gate_bT{l}", [128, 24]) for l in range(L)]
        self.gm_wsT = [di(f"gm_wsT{l}", [128, 512]) for l in range(L)]
        self.gm_bsT = [di(f"gm_bsT{l}", [128, 4]) for l in range(L)]
        self.gm_ln = [di(f"gm_ln{l}", [2, 512]) for l in range(L)]
        self.w_branch = [di(f"w_branch{l}", [3, 512, D]) for l in range(L)]
        self.w_out = [di(f"w_out{l}", [D, D]) for l in range(L)]
        self.ln_gb = [di(f"ln_gb{l}", [4, D]) for l in range(L)]
        self.dense_up = di("dense_up", [D, 2 * FF_DENSE])
        self.dense_down = di("dense_down", [FF_DENSE, D])
        if L > 1:
            self.router = di("router", [D, NEXP])
            self.moe_up = di("moe_up", [NEXP, D, 2 * FF_EXP])
            self.moe_down = di("moe_down", [NEXP, FF_EXP, D])
        self.y = nc.dram_tensor("y", [S, D], F32, kind="ExternalOutput").ap()
        self.xres = nc.dram_tensor("xres", [S, D], F32).ap()
        nbytes = (int(nc.sbuf_bytes_remaining) // 64) * 64 - 64
        self.arena_bytes = nbytes
        self.arena = nc.alloc_sbuf_tensor("arena", [128, nbytes], U8)
        self.psum = nc.alloc_psum_tensor("psum", [128, 4096], F32)
        self.off = 0

    def din(self, name, shape):
        t = self.nc.dram_tensor(name, list(shape), F32, kind="ExternalInput").ap()
        self.inputs[name] = tuple(shape)
        return t

    def A(self, shape, dt):
        esz = 4 if dt == F32 else 2
        n = int(np.prod(shape[1:]))
        nb = n * esz
        off = self.off
        assert off + nb <= self.arena_bytes, ("SBUF arena overflow", off, nb, self.arena_bytes)
        ap = self.arena[:, off:off + nb].bitcast(dt)
        if len(shape) == 3:
            ap = ap.rearrange("p (a b) -> p a b", a=shape[1])
        elif len(shape) == 4:
            ap = ap.rearrange("p (a b c) -> p a b c", a=shape[1], b=shape[2])
        self.off = off + ((nb + 63) // 64) * 64
        return ap

    def Vat(self, off, shape, dt):
        save = self.off
        self.off = off
        ap = self.A(shape, dt)
        self.off = save
        return ap

    def prefetch_AB(self, l):
        w_in = self.w_in[l].rearrange("(kc p) n -> p kc n", p=128)
        wA = self.Vat(self.SLOT[0], [128, KC, 1536], BF16)
        wB = self.Vat(self.SLOT[1], [128, KC, 1544], BF16)
        self.dma(self.pool, out=wA, in_=w_in[:, :, 0:1536], writes=["slot0"], sem="slot0")
        self.dma(self.pool, out=wB, in_=w_in[:, :, 1536:3080], writes=["slot1"], sem="slot1")

    def bank(self, b, dt=F32):
        ap = self.psum[:, b * 512:(b + 1) * 512]
        if dt == BF16:
            ap = ap.bitcast(BF16)
        return ap

    def R(self, k):
        r = self.res.get(k)
        if r is None:
            r = self.res[k] = Res()
        return r

    def dsem(self, name):
        s = self.dsems.get(name)
        if s is None:
            s = self.dsems[name] = DSem(self.nc, name)
        return s

    def wait(self, e, so, v):
        if v <= 0 or e.seen.get(so, 0) >= v:
            return
        if self.pending is not None:
            cur = self.pending.get(so, 0)
            if v > cur:
                self.pending[so] = v
            return
        e.eng.wait_ge(so.sem, v)
        e.seen[so] = v

    def flush_waits(self, e, keep_one):
        items = [(so, v) for so, v in self.pending.items() if e.seen.get(so, 0) < v]
        self.pending = None
        last = None
        if keep_one and items:
            last = items.pop()
        for so, v in items:
            e.eng.wait_ge(so.sem, v)
            e.seen[so] = v
        return last

    def _deps(self, e, reads, writes):
        for k in reads:
            R = self.R(k)
            if R.w is not None:
                so, v = R.w
                if not (so is e and e is self.pe):
                    self.wait(e, so, v)
        for k in writes:
            R = self.R(k)
            if R.w is not None:
                so, v = R.w
                if so is not e:
                    self.wait(e, so, v)
            for so, v in R.r.items():
                if so is not e:
                    self.wait(e, so, v)

    def op(self, e, fn, reads=(), writes=(), inc=True):
        if e is not self.pe:
            extra = [k for k in reads if isinstance(k, str) and k[0] == "B" and k[1:].isdigit() and k not in writes]
            if extra:
                writes = list(writes) + extra
        self.pending = {}
        self._deps(e, reads, writes)
        last = self.flush_waits(e, keep_one=(e is not self.pe))
        ins = fn()
        if last is not None:
            ins._wait_ge(last[0].sem, last[1])
            e.seen[last[0]] = last[1]
        if inc:
            ins.then_inc(e.sem, 1)
            e.cnt += 1
            tag = e.cnt
        else:
            tag = e.cnt + 1
        for k in reads:
            R = self.R(k)
            R.r[e] = max(R.r.get(e, 0), tag)
        for k in writes:
            R = self.R(k)
            R.w = (e, tag)
            R.r = {}
        return ins

    def dma(self, q, out, in_, reads=(), writes=(), sem="misc", **kw):
        Sm = self.dsem(sem)
        self._deps(q, reads, writes)
        q.eng.dma_start(out=out, in_=in_, **kw).then_inc(Sm.sem, 16)
        Sm.cnt += 16
        for k in reads:
            self.R(k).r[Sm] = Sm.cnt
        for k in writes:
            R = self.R(k)
            R.w = (Sm, Sm.cnt)
            R.r = {}

    def barrier(self):
        for e in self.engs:
            for o in self.engs:
                if o is not e:
                    self.wait(e, o, o.cnt)
            for s in self.dsems.values():
                self.wait(e, s, s.cnt)

    def tap(self, name, ap, shape, reads=()):
        if not self.debug:
            return
        t = self.nc.dram_tensor("dbg_" + name, list(shape), ap.dtype, kind="ExternalOutput").ap()
        self.dma(self.sp, out=t, in_=ap, reads=reads, sem="dbg")
        self.taps.append("dbg_" + name)

    def mm(self, out, lhsT, rhs, start, stop, reads=(), writes=(), inc=None):
        if inc is None:
            inc = stop
        return self.op(self.pe, lambda: self.nc.tensor.matmul(out, lhsT=lhsT, rhs=rhs, start=start, stop=stop),
                       reads, writes, inc)

    def tr(self, out, in_, ident, reads=(), writes=(), inc=True):
        return self.op(self.pe, lambda: self.nc.tensor.transpose(out=out, in_=in_, identity=ident),
                       reads, writes, inc)

    def actf(self, out, in_, func, reads=(), writes=(), **kw):
        return self.op(self.act, lambda: self.nc.scalar.activation(out=out, in_=in_, func=func, **kw), reads, writes)

    def tt(self, out, in0, in1, op, reads=(), writes=()):
        return self.op(self.dve, lambda: self.nc.vector.tensor_tensor(out=out, in0=in0, in1=in1, op=op), reads, writes)

    def ts(self, out, in0, s1, s2, op0, op1=None, reads=(), writes=()):
        if op1 is None:
            return self.op(self.dve, lambda: self.nc.vector.tensor_scalar(out=out, in0=in0, scalar1=s1, scalar2=None, op0=op0),
                           reads, writes)
        return self.op(self.dve, lambda: self.nc.vector.tensor_scalar(out=out, in0=in0, scalar1=s1, scalar2=s2, op0=op0, op1=op1),
                       reads, writes)

    def stt(self, out, in0, scalar, in1, op0, op1, reads=(), writes=()):
        return self.op(self.dve, lambda: self.nc.vector.scalar_tensor_tensor(out=out, in0=in0, scalar=scalar, in1=in1, op0=op0, op1=op1),
                       reads, writes)

    def vcopy(self, out, in_, reads=(), writes=()):
        return self.op(self.dve, lambda: self.nc.vector.tensor_copy(out=out, in_=in_), reads, writes)

    def ln_stats(self, src, n, eps, key_src, tmp, ks=""):
        st, mv, rstd = tmp
        nch = (n + 511) // 512
        for c in range(nch):
            w = min(512, n - c * 512)
            self.op(self.dve, lambda c=c, w=w: self.nc.vector.bn_stats(out=st[:, c, :], in_=src[:, c * 512:c * 512 + w]),
                    reads=[key_src], writes=["ln_st" + ks])
        self.op(self.dve, lambda: self.nc.vector.bn_aggr(out=mv, in_=st[:, 0:nch, :].rearrange("p a b -> p (a b)")),
                reads=["ln_st" + ks], writes=["ln_mv" + ks])
        self.actf(rstd, mv[:, 1:2], AF.Sqrt, reads=["ln_mv" + ks], writes=["ln_rstd" + ks], bias=eps, scale=1.0)
        self.op(self.dve, lambda: self.nc.vector.reciprocal(out=rstd, in_=rstd), reads=["ln_rstd" + ks], writes=["ln_rstd" + ks])
        return mv, rstd

    def interleave(self, genf, n, width=2, lag=3):
        active = []
        nxt = 0
        while nxt < n or active:
            if nxt < n and len(active) < width and (not active or active[-1][1] >= lag):
                active.append([genf(nxt), 0])
                nxt += 1
            for a in list(active):
                try:
                    next(a[0])
                    a[1] += 1
                except StopIteration:
                    active.remove(a)

    def build(self):
        nc = self.nc
        pe, act, dve, pool, sp = self.pe, self.act, self.dve, self.pool, self.sp
        self.xT = self.A([128, KC, S], BF16)
        self.cf = self.A([128, NCF - NCF1], F32)
        self.cb = self.A([128, NCB], BF16)
        cf, cb = self.cf, self.cb
        o_ = NCF1
        self.pmask = cf[:, CF_SMALL - o_:CF_SMALL - o_ + 2]
        self.sdec = cf[:, CF_SMALL - o_ + 2:CF_SMALL - o_ + 4]
        self.tri = cf[:, CF_TRI - o_:CF_TRI - o_ + 128]
        self.identf = cf[:, CF_IDENT - o_:CF_IDENT - o_ + 128]
        self.negm = cf[:, CF_NEGM - o_:CF_NEGM - o_ + 128]
        self.onesf = cf[:, CF_ONES - o_:CF_ONES - o_ + 128]
        self.hmask = cb[:, CB_HMASK:CB_HMASK + 1024].rearrange("p (a b) -> p a b", a=8)
        self.mask01 = cb[:, CB_MASK:CB_MASK + 128]
        self.identb = cb[:, CB_IDENT:CB_IDENT + 128]
        self.lng = self.A([128, D], F32)
        self.lnb = self.A([128, D], F32)
        self.gates = self.A([128, NT, NEXP], F32)
        self.st = self.A([128, 4, 6], F32)
        self.mv = self.A([128, 4, 2], F32)
        self.rstd = self.A([128, 4], F32)
        self.lnst = self.A([128, 2, 6], F32)
        self.lnmv = self.A([128, 2], F32)
        self.lnrstd = self.A([128, 1], F32)
        self.lnt = [(self.A([128, 2, 6], F32), self.A([128, 2], F32), self.A([128, 1], F32)) for _ in range(2)]
        self.gnt = [(self.A([128, 4, 6], F32), self.A([128, 4, 2], F32), self.A([128, 4], F32)) for _ in range(2)]
        self.MBASE = self.off
        self.SLOT = [self.MBASE + 107520, self.MBASE + 107520 + 24704]
        assert self.SLOT[1] + 24704 <= self.arena_bytes, (self.MBASE, self.arena_bytes)
        self.dma(sp, out=cf, in_=self.constf[:, NCF1:NCF], writes=["cf"], sem="const")
        self.dma(pool, out=cb, in_=self.constb[:, :], writes=["cb"], sem="const")

        self.off = self.MBASE
        xbs = [self.A([128, D], BF16) for _ in range(2)]
        for i in range(NT):
            b = i % 2
            self.dma(pool, out=xbs[b], in_=self.x[i * 128:(i + 1) * 128, :], writes=[f"xb{b}"], sem=f"xb{b}")
            psT = self.bank(b, BF16).rearrange("p (a b) -> p a b", a=8)
            for kc in range(KC):
                self.tr(psT[:, kc, :], xbs[b][:, kc * 128:(kc + 1) * 128], self.identb,
                        reads=[f"xb{b}", "cb"], writes=[f"ps{b}"], inc=(kc == KC - 1))
            self.actf(self.xT[:, :, i * 128:(i + 1) * 128], psT, AF.Copy, reads=[f"ps{b}"], writes=[("xT", i)])
        self.barrier()
        if self.stop_after == ("P0", 0):
            self.tap("xT", self.xT, [128, KC, S])
            self.barrier()
            return nc

        for l in range(self.n_layers):
            try:
                self.mixer(l)
            except Cut:
                self.barrier()
                return nc
            if self.stop_after is not None and self.stop_after[0] in ("P1", "P2", "P3", "P4", "P5") and self.stop_after[1] == l:
                break
            if self.stop_after == ("mixer", l):
                break
            self.ffn(l)
            if self.stop_after == ("ffn", l):
                break
        self.barrier()
        return nc

    def mixer(self, l):
        nc = self.nc
        pe, act, dve, pool, sp = self.pe, self.act, self.dve, self.pool, self.sp
        xT = self.xT
        w_in = self.w_in[l].rearrange("(kc p) n -> p kc n", p=128)
        self.off = self.MBASE
        YT = self.A([128, 12, S], BF16)
        P_OFF = self.off
        B = [self.bank(b) for b in range(8)]
        Bb = [self.bank(b, BF16) for b in range(8)]

        if l == 0:
            self.prefetch_AB(0)
        wA = self.Vat(self.SLOT[0], [128, KC, 1536], BF16)
        cf1 = self.A([128, NCF1], F32)
        self.dma(sp, out=cf1, in_=self.constf[:, 0:NCF1], writes=["cf1"], sem="cf1")
        self.cosv = cf1[:, CF_COS:CF_COS + 512].rearrange("p (a b) -> p a b", a=NT)
        self.sinv = cf1[:, CF_SIN:CF_SIN + 512].rearrange("p (a b) -> p a b", a=NT)
        self.decin = cf1[:, CF_DECIN:CF_DECIN + 512]
        self.kdm = cf1[:, CF_KDM:CF_KDM + 512].rearrange("p (a b) -> p a b", a=4)
        self.qdec = cf1[:, CF_QDEC:CF_QDEC + 256]

        def p1set():
            return dict(rot=self.A([128, 8, 2, 32], F32), t1=self.A([128, 8, 32], F32), t2=self.A([128, 8, 32], F32),
                        rot_bf=self.A([128, 512], BF16), qp_bf=self.A([128, 256], BF16), kpm=self.A([128, 4, 128], BF16),
                        qTm=self.A([128, 4, 128], BF16), qpTm=self.A([128, 4, 128], BF16), kT=self.A([128, 2, 128], BF16),
                        ST=self.A([128, 4, 128], BF16), v_bf=self.A([128, 512], BF16), yn=self.A([128, 512], F32),
                        sg=self.A([128, 512], F32), ya=self.A([128, 512], BF16))
        p1s = [p1set(), p1set()]
        assert self.off <= self.SLOT[0], ("P1 locals overlap weight slots", self.off, self.SLOT)
        Sst = self.A([128, 2, 128], F32)
        Sbf = self.A([128, 2, 128], BF16)
        self.op(dve, lambda: nc.vector.memset(Sst, 0.0), writes=["Sst"])
        self.op(dve, lambda: nc.vector.memset(Sbf, 0.0), writes=["Sbf"])
        p1_done = {"n": 0}

        def p1gen(i):
            tsl = slice(i * 128, (i + 1) * 128)
            par = i % 2
            T = p1s[par]
            k = lambda nm: f"{nm}{par}"
            X = [4 * par + j for j in range(4)]
            bk = lambda j: f"B{X[j]}"
            rot, t1, t2, rot_bf, qp_bf, kpm = T["rot"], T["t1"], T["t2"], T["rot_bf"], T["qp_bf"], T["kpm"]
            qTm, qpTm, kT, ST, v_bf, yn, sg, ya = T["qTm"], T["qpTm"], T["kT"], T["ST"], T["v_bf"], T["yn"], T["sg"], T["ya"]
            gst, gmv, grs = self.gnt[par]
            for c in range(3):
                for kc in range(KC):
                    self.mm(B[X[c]], xT[:, kc, tsl], wA[:, kc, c * 512:(c + 1) * 512], kc == 0, kc == KC - 1,
                            reads=["slot0"], writes=[bk(c)])
            yield
            qk = B[X[0]].rearrange("p (g h d) -> p g h d", g=8, h=2)
            x1, x2 = qk[:, :, 0, :], qk[:, :, 1, :]
            cb_ = self.cosv[:, i, :].unsqueeze(1).broadcast_to([128, 8, 32])
            sb_ = self.sinv[:, i, :].unsqueeze(1).broadcast_to([128, 8, 32])
            self.tt(t1, x1, cb_, ALU.mult, reads=[bk(0), "cf1"], writes=[k("t1")])
            self.tt(t2, x2, sb_, ALU.mult, reads=[bk(0)], writes=[k("t2")])
            self.actf(v_bf, B[X[1]], AF.Copy, reads=[bk(1)], writes=[k("v_bf")])
            yield
            self.tt(rot[:, :, 0, :], t1, t2, ALU.subtract, reads=[k("t1"), k("t2")], writes=[k("rot0")])
            self.tt(t1, x1, sb_, ALU.mult, reads=[bk(0)], writes=[k("t1")])
            self.actf(sg, B[X[2]], AF.Silu, reads=[bk(2)], writes=[k("sg")])
            yield
            self.tt(t2, x2, cb_, ALU.mult, reads=[bk(0)], writes=[k("t2")])
            self.tt(rot[:, :, 1, :], t1, t2, ALU.add, reads=[k("t1"), k("t2")], writes=[k("rot1")])
            yield
            rotf = rot.rearrange("p g h d -> p (g h d)")
            self.actf(rot_bf, rotf, AF.Copy, reads=[k("rot0"), k("rot1")], writes=[k("rot_bf")])
            self.tt(qp_bf, rotf[:, 0:256], self.qdec, ALU.mult, reads=[k("rot0"), k("rot1")], writes=[k("qp_bf")])
            yield
            for h in range(4):
                pr = h // 2
                self.tt(kpm[:, h, :], rotf[:, 256 + pr * 128:256 + (pr + 1) * 128], self.kdm[:, h, :], ALU.mult,
                        reads=[k("rot0"), k("rot1")], writes=[k("kpm")])
            yield
            psT = Bb[X[3]].rearrange("p (a b) -> p a b", a=8)
            srcs = [rot_bf[:, 0:128], rot_bf[:, 128:256], rot_bf[:, 256:384], rot_bf[:, 384:512],
                    qp_bf[:, 0:128], qp_bf[:, 128:256]]
            for k_, s_ in enumerate(srcs):
                self.tr(psT[:, k_, :], s_, self.identb, reads=[k("rot_bf"), k("qp_bf"), "cb"], writes=[bk(3)], inc=(k_ == 5))
            yield
            qTm4 = qTm.rearrange("p (a b) t -> p a b t", a=2)
            qpTm4 = qpTm.rearrange("p (a b) t -> p a b t", a=2)
            for pr_ in range(2):
                self.ts(qTm4[:, :, pr_, :], psT[:, 0:2, :], self.pmask[:, pr_:pr_ + 1], None, ALU.mult,
                        reads=[bk(3)], writes=[k("qTm")])
            self.actf(kT, psT[:, 2:4, :], AF.Copy, reads=[bk(3)], writes=[k("kT")])
            yield
            for pr_ in range(2):
                self.ts(qpTm4[:, :, pr_, :], psT[:, 4:6, :], self.pmask[:, pr_:pr_ + 1], None, ALU.mult,
                        reads=[bk(3)], writes=[k("qpTm")])
            yield
            for h in range(4):
                self.mm(B[X[0]][:, h * 128:(h + 1) * 128], kT[:, h // 2, :], qTm[:, h, :], True, True,
                        reads=[k("kT"), k("qTm")], writes=[bk(0)], inc=(h == 3))
            yield
            self.tt(ST.rearrange("p a b -> p (a b)"), B[X[0]], self.decin, ALU.mult, reads=[bk(0)], writes=[k("ST")])
            yield
            while p1_done["n"] < i:
                yield
            for h in range(4):
                self.mm(B[X[1]][:, h * 128:(h + 1) * 128], ST[:, h, :], v_bf[:, h * 128:(h + 1) * 128], True, False,
                        reads=[k("ST"), k("v_bf")], writes=[bk(1)], inc=False)
                self.mm(B[X[1]][:, h * 128:(h + 1) * 128], qpTm[:, h, :], Sbf[:, h // 2, :], False, True,
                        reads=[k("qpTm"), "Sbf"], writes=[bk(1)], inc=(h == 3))
            for pr in range(2):
                for hl in range(2):
                    h = 2 * pr + hl
                    self.mm(B[X[2]][:, pr * 128:(pr + 1) * 128], kpm[:, h, :], v_bf[:, h * 128:(h + 1) * 128],
                            hl == 0, hl == 1, reads=[k("kpm"), k("v_bf")], writes=[bk(2)], inc=(pr == 1 and hl == 1))
            yield
            for pr in range(2):
                self.stt(Sst[:, pr, :], Sst[:, pr, :], self.sdec[:, pr:pr + 1], B[X[2]][:, pr * 128:(pr + 1) * 128],
                         ALU.mult, ALU.add, reads=["Sst", bk(2)], writes=["Sst"])
            self.actf(Sbf, Sst, AF.Copy, reads=["Sst"], writes=["Sbf"])
            p1_done["n"] = i + 1
            yield
            for h in range(4):
                self.op(dve, lambda h=h: nc.vector.bn_stats(out=gst[:, h, :], in_=B[X[1]][:, h * 128:(h + 1) * 128]),
                        reads=[bk(1)], writes=[k("gst")])
            yield
            for h in range(4):
                self.op(dve, lambda h=h: nc.vector.bn_aggr(out=gmv[:, h, :], in_=gst[:, h, :]),
                        reads=[k("gst")], writes=[k("gmv")])
            self.actf(grs, gmv[:, :, 1], AF.Sqrt, reads=[k("gmv")], writes=[k("grs")], bias=GN_EPS, scale=1.0)
            yield
            self.op(dve, lambda: nc.vector.reciprocal(out=grs, in_=grs), reads=[k("grs")], writes=[k("grs")])
            yield
            for h in range(4):
                self.ts(yn[:, h * 128:(h + 1) * 128], B[X[1]][:, h * 128:(h + 1) * 128], gmv[:, h, 0:1],
                        grs[:, h:h + 1], ALU.subtract, ALU.mult, reads=[bk(1), k("gmv"), k("grs")], writes=[k("yn")])
            yield
            self.tt(ya, yn, sg, ALU.mult, reads=[k("yn"), k("sg")], writes=[k("ya")])
            yield
            psY = Bb[X[0]].rearrange("p (a b) -> p a b", a=8)
            for k_ in range(4):
                self.tr(psY[:, k_, :], ya[:, k_ * 128:(k_ + 1) * 128], self.identb, reads=[k("ya")], writes=[bk(0)], inc=(k_ == 3))
            yield
            self.actf(YT[:, 0:4, tsl], psY[:, 0:4, :], AF.Copy, reads=[bk(0)], writes=[("YT", i)])

        self.interleave(p1gen, NT, width=2, lag=8)
        self.barrier()
        if self.stop_after == ("P1", l):
            self.tap("YT", YT, [128, 12, S])
            return

        self.off = P_OFF
        wB = self.Vat(self.SLOT[1], [128, KC, 1544], BF16)
        wC = self.Vat(self.SLOT[0], [128, KC, 1024], BF16)
        self.dma(pool, out=wC, in_=w_in[:, :, 3080:4104], writes=["slot0"], sem="slot0")
        foxb = self.A([128, 8], F32)
        self.dma(sp, out=foxb, in_=self.fox_b[l][0:1, :].partition_broadcast(128), writes=["foxb"], sem="small")
        KT = self.A([128, 4, S], BF16)
        KBc = self.A([128, S], BF16)
        Vaug = self.A([128, NT, 8, 65], BF16)
        q_bf = self.A([128, 512], BF16)
        k_bf = self.A([128, 512], BF16)
        z = self.A([128, 8], F32)
        ez = self.A([128, 8], F32)
        lf = self.A([128, 8], F32)
        negc = self.A([128, 8], F32)
        carry = self.A([128, 8], F32)
        r1 = self.A([128, 8], F32)
        r2 = self.A([128, 8], F32)
        BK = self.A([128, 128], BF16)
        BQ = self.A([128, 128], BF16)
        qTm8s = [self.A([128, 8, 128], BF16) for _ in range(2)]
        QBms = [self.A([128, 8, 128], BF16) for _ in range(2)]
        PT = [self.A([128, NT, 128], BF16) for _ in range(2)]
        rs = self.A([128, 8], F32)
        yb = self.A([128, 8, 64], BF16)
        assert self.off <= self.SLOT[0], ("P2 locals overlap weight slots", self.off, self.SLOT)
        self.op(dve, lambda: nc.vector.memset(carry, 0.0), writes=["carry"])
        self.op(dve, lambda: nc.vector.memset(BK, 0.0), writes=["BK"])
        self.op(dve, lambda: nc.vector.memset(BQ, 0.0), writes=["BQ"])
        self.op(dve, lambda: nc.vector.memset(Vaug.rearrange("p a b c -> p (a b c)"), 1.0), writes=["Vaug"])
        BK3 = BK[:, 0:48].rearrange("p (h r) -> p h r", h=8)
        BQ3 = BQ[:, 0:48].rearrange("p (h r) -> p h r", h=8)
        self.op(dve, lambda: nc.vector.memset(BK3[:, :, 3:6], 1.0), reads=[], writes=["BK"])
        self.op(dve, lambda: nc.vector.memset(BQ3[:, :, 0:3], 1.0), reads=[], writes=["BQ"])
        psT1 = Bb[2].rearrange("p (a b) -> p a b", a=8)
        psT2 = Bb[3].rearrange("p (a b) -> p a b", a=8)
        st8 = {"chunk": 0}

        def chain_stages(i):
            tsl = slice(i * 128, (i + 1) * 128)
            par = i % 2
            qTm8, QBm = qTm8s[par], QBms[par]
            qk_, qb_ = f"qTm8_{par}", f"QBm_{par}"

            def c1():
                for kc in range(KC):
                    self.mm(B[0], xT[:, kc, tsl], wB[:, kc, 0:512], kc == 0, kc == KC - 1, reads=["slot1"], writes=["B0"])
                for kc in range(KC):
                    self.mm(B[1], xT[:, kc, tsl], wB[:, kc, 512:1024], kc == 0, kc == KC - 1, reads=["slot1"], writes=["B1"])

            def c2():
                self.actf(q_bf, B[0], AF.Copy, reads=["B0"], writes=["q_bf"])
                self.actf(k_bf, B[1], AF.Identity, reads=["B1"], writes=["k_bf"], scale=0.125)

            def c3():
                for kc in range(KC):
                    self.mm(B[0], xT[:, kc, tsl], wB[:, kc, 1024:1536], kc == 0, kc == KC - 1, reads=["slot1"], writes=["B0"])
                for kc in range(KC):
                    self.mm(B[1][:, 0:8], xT[:, kc, tsl], wB[:, kc, 1536:1544], kc == 0, kc == KC - 1, reads=["slot1"], writes=["B1"])
                for k_ in range(4):
                    self.tr(psT1[:, k_, :], q_bf[:, k_ * 128:(k_ + 1) * 128], self.identb, reads=["q_bf", "cb"], writes=["B2"], inc=False)
                for k_ in range(4):
                    self.tr(psT1[:, 4 + k_, :], k_bf[:, k_ * 128:(k_ + 1) * 128], self.identb, reads=["k_bf"], writes=["B2"], inc=(k_ == 3))

            def c4():
                self.actf(Vaug[:, i, :, 0:64], B[0].rearrange("p (h d) -> p h d", h=8), AF.Copy, reads=["B0"], writes=[("V", i)])
                self.tt(z, B[1][:, 0:8], foxb, ALU.add, reads=["B1", "foxb"], writes=["z"])
                self.actf(ez, z, AF.Exp, reads=["z"], writes=["ez"], scale=-1.0)
                self.actf(lf, ez, AF.Ln, reads=["ez"], writes=["lf"], bias=1.0, scale=1.0)
                qTm84 = qTm8.rearrange("p (a b) t -> p a b t", a=4)
                for pr_ in range(2):
                    self.ts(qTm84[:, :, pr_, :], psT1[:, 0:4, :], self.pmask[:, pr_:pr_ + 1], None, ALU.mult,
                            reads=["B2"], writes=[qk_])
                self.actf(KT[:, :, tsl], psT1[:, 4:8, :], AF.Copy, reads=["B2"], writes=[("KT", i)])

            def c5():
                self.mm(B[1][:, 8:16], self.tri, lf, True, True, reads=["lf", "cf"], writes=["B1"], inc=False)
                self.mm(B[1][:, 16:24], self.onesf, lf, True, True, reads=["lf"], writes=["B1"], inc=True)

            def c6():
                self.tt(negc, B[1][:, 8:16], carry, ALU.add, reads=["B1", "carry"], writes=["negc"])
                self.tt(carry, B[1][:, 16:24], carry, ALU.add, reads=["B1", "carry"], writes=["carry"])
                self.vcopy(BK3[:, :, 0], negc, reads=["negc"], writes=["BKa"])
                self.tt(r1, negc, BK3[:, :, 0], ALU.subtract, reads=["negc", "BKa"], writes=["r1"])
                self.vcopy(BK3[:, :, 1], r1, reads=["r1"], writes=["BKb"])
                self.tt(r2, r1, BK3[:, :, 1], ALU.subtract, reads=["r1", "BKb"], writes=["r2"])
                self.vcopy(BK3[:, :, 2], r2, reads=["r2"], writes=["BKc"])
                self.ts(BQ3[:, :, 3:6], BK3[:, :, 0:3], -1.0, None, ALU.mult, reads=["BKa", "BKb", "BKc"], writes=["BQ"])

            def c7():
                self.tr(psT2[:, 0, :], BK, self.identb, reads=["BK", "BKa", "BKb", "BKc"], writes=["B3"], inc=False)
                self.tr(psT2[:, 1, :], BQ, self.identb, reads=["BQ"], writes=["B3"], inc=True)

            def c8():
                self.actf(KBc[:, tsl], psT2[:, 0, :], AF.Copy, reads=["B3"], writes=[("KB", i)])
                self.tt(QBm, psT2[:, 1, :].unsqueeze(1).broadcast_to([128, 8, 128]), self.hmask, ALU.mult,
                        reads=["B3", "cb"], writes=[qb_])
            return [c1, c2, c3, c4, c5, c6, c7, c8]

        def head(i, h):
            par = i % 2
            qTm8, QBm = qTm8s[par], QBms[par]
            qk_, qb_ = f"qTm8_{par}", f"QBm_{par}"
            PTb = PT[h % 2]
            pk = f"PT{h % 2}"
            for c0 in range(0, i + 1, 4):
                nb = min(4, i + 1 - c0)
                bk = 4 + (st8["chunk"] % 2)
                st8["chunk"] += 1
                for jj in range(nb):
                    j = c0 + jj
                    self.mm(B[bk][:, jj * 128:(jj + 1) * 128], KT[:, h // 2, j * 128:(j + 1) * 128], qTm8[:, h, :],
                            True, False, reads=[("KT", j), qk_], writes=[f"B{bk}"], inc=False)
                    self.mm(B[bk][:, jj * 128:(jj + 1) * 128], KBc[:, j * 128:(j + 1) * 128], QBm[:, h, :],
                            False, True, reads=[("KB", j), qb_], writes=[f"B{bk}"], inc=(jj == nb - 1))
                if c0 + nb == i + 1:
                    jj = nb - 1
                    self.tt(B[bk][:, jj * 128:(jj + 1) * 128], B[bk][:, jj * 128:(jj + 1) * 128], self.negm, ALU.add,
                            reads=[f"B{bk}"], writes=[f"B{bk}"])
                self.actf(PTb[:, c0:c0 + nb, :].rearrange("p a b -> p (a b)"), B[bk][:, 0:nb * 128], AF.Exp,
                          reads=[f"B{bk}"], writes=[pk])

        def head_pv(i, h):
            PTb = PT[h % 2]
            pk = f"PT{h % 2}"
            ob = 6 + h // 4
            for j in range(i + 1):
                self.mm(B[ob][:, (h % 4) * 65:(h % 4) * 65 + 65], PTb[:, j, :], Vaug[:, j, h, :], j == 0, j == i,
                        reads=[pk, ("V", j), "Vaug"], writes=[f"B{ob}"], inc=(j == i))

        def finish(i):
            tsl = slice(i * 128, (i + 1) * 128)
            for hb in range(2):
                po = B[6 + hb][:, 0:260].rearrange("p (h c) -> p h c", h=4)
                self.op(dve, lambda po=po, hb=hb: nc.vector.reciprocal(out=rs[:, hb * 4:(hb + 1) * 4], in_=po[:, :, 64]),
                        reads=[f"B{6 + hb}"], writes=["rs"])
                self.tt(yb[:, hb * 4:(hb + 1) * 4, :], po[:, :, 0:64],
                        rs[:, hb * 4:(hb + 1) * 4].unsqueeze(2).broadcast_to([128, 4, 64]), ALU.mult,
                        reads=[f"B{6 + hb}", "rs"], writes=["yb"])
            ybf = yb.rearrange("p a b -> p (a b)")
            for k_ in range(4):
                self.tr(psT2[:, 2 + k_, :], ybf[:, k_ * 128:(k_ + 1) * 128], self.identb, reads=["yb"], writes=["B3"], inc=(k_ == 3))
            self.actf(YT[:, 4:8, tsl], psT2[:, 2:6, :], AF.Copy, reads=["B3"], writes=[("YT", i)])

        for c_ in chain_stages(0):
            c_()
        for i in range(NT):
            nxt = chain_stages(i + 1) if i + 1 < NT else []
            for h in range(8):
                if h < len(nxt):
                    nxt[h]()
                head(i, h)
                if h >= 1:
                    head_pv(i, h - 1)
            head_pv(i, 7)
            finish(i)
        self.barrier()
        if self.stop_after == ("P2", l):
            self.tap("YT", YT, [128, 12, S])
            return

        self.off = P_OFF
        wO = self.Vat(self.SLOT[1], [128, KC, D], BF16)
        self.dma(pool, out=wO, in_=self.w_out[l].rearrange("(kc p) n -> p kc n", p=128), writes=["slot1"], sem="slot1")
        wsf = self.A([128, 512], F32)
        WST = self.A([128, 4, 128], BF16)
        bsT = self.A([128, 4], F32)
        gmg = self.A([128, 512], F32)
        gmb = self.A([128, 512], F32)
        self.dma(sp, out=wsf, in_=self.gm_wsT[l][:, :], writes=["wsf"], sem="small")
        self.dma(sp, out=bsT, in_=self.gm_bsT[l][:, :], writes=["bsT"], sem="small")
        self.dma(sp, out=gmg, in_=self.gm_ln[l][0:1, :].partition_broadcast(128), writes=["gmg"], sem="small")
        self.dma(sp, out=gmb, in_=self.gm_ln[l][1:2, :].partition_broadcast(128), writes=["gmb"], sem="small")
        for r_ in ("wsf", "bsT", "gmg", "gmb", "foxb"):
            self.R(r_).w = (self.dsem("small"), self.dsem("small").cnt)
        self.tt(WST, wsf.rearrange("p (a b) -> p a b", a=4), self.mask01.unsqueeze(1).broadcast_to([128, 4, 128]),
                ALU.mult, reads=["wsf", "cb"], writes=["WST"])
        us = [self.A([128, 512], F32) for _ in range(2)]
        gvs = [self.A([128, 512], F32) for _ in range(2)]
        vbs = [self.A([128, 512], BF16) for _ in range(2)]
        ycs = [self.A([128, 512], BF16) for _ in range(2)]

        def p3gen(i):
            tsl = slice(i * 128, (i + 1) * 128)
            par = i % 2
            X = [4 * par + j for j in range(4)]
            bk = lambda j: f"B{X[j]}"
            u, gvv, vb, yc = us[par], gvs[par], vbs[par], ycs[par]
            gk, ks = f"gvv{par}", f"_{par}"
            for c in range(2):
                for kc in range(KC):
                    self.mm(B[X[c]], xT[:, kc, tsl], wC[:, kc, c * 512:(c + 1) * 512], kc == 0, kc == KC - 1,
                            reads=["slot0"], writes=[bk(c)])
            yield
            self.actf(gvv, B[X[1]], AF.Gelu_apprx_tanh, reads=[bk(1)], writes=[gk])
            self.actf(u, B[X[0]], AF.Gelu_apprx_tanh, reads=[bk(0)], writes=[f"u{par}"])
            yield
            st_, mv_, rs_ = self.lnt[par]
            self.op(dve, lambda: nc.vector.bn_stats(out=st_[:, 0, :], in_=gvv), reads=[gk], writes=["ln_st" + ks])
            yield
            self.op(dve, lambda: nc.vector.bn_aggr(out=mv_, in_=st_[:, 0, :]), reads=["ln_st" + ks], writes=["ln_mv" + ks])
            yield
            self.actf(rs_, mv_[:, 1:2], AF.Sqrt, reads=["ln_mv" + ks], writes=["ln_rstd" + ks], bias=LN_EPS, scale=1.0)
            yield
            self.op(dve, lambda: nc.vector.reciprocal(out=rs_, in_=rs_), reads=["ln_rstd" + ks], writes=["ln_rstd" + ks])
            yield
            self.ts(gvv, gvv, mv_[:, 0:1], rs_[:, 0:1], ALU.subtract, ALU.mult, reads=[gk, "ln_mv" + ks, "ln_rstd" + ks], writes=[gk])
            yield
            self.tt(gvv, gvv, gmg, ALU.mult, reads=[gk, "gmg"], writes=[gk])
            yield
            self.tt(vb, gvv, gmb, ALU.add, reads=[gk, "gmb"], writes=[f"vb{par}"])
            yield
            for g in range(4):
                self.mm(B[X[2]][:, g * 128:(g + 1) * 128], WST[:, g, :], vb[:, g * 128:(g + 1) * 128], True, True,
                        reads=["WST", f"vb{par}"], writes=[bk(2)], inc=(g == 3))
            yield
            for g in range(4):
                self.stt(yc[:, g * 128:(g + 1) * 128], B[X[2]][:, g * 128:(g + 1) * 128], bsT[:, g:g + 1],
                         u[:, g * 128:(g + 1) * 128], ALU.add, ALU.mult, reads=[bk(2), "bsT", f"u{par}"], writes=[f"yc{par}"])
            yield
            psT = Bb[X[3]].rearrange("p (a b) -> p a b", a=8)
            for k_ in range(4):
                self.tr(psT[:, k_, :], yc[:, k_ * 128:(k_ + 1) * 128], self.identb, reads=[f"yc{par}"], writes=[bk(3)], inc=(k_ == 3))
            yield
            self.actf(YT[:, 8:12, tsl], psT[:, 0:4, :], AF.Copy, reads=[bk(3)], writes=[("YT", i)])

        self.interleave(p3gen, NT, width=2, lag=5)
        self.barrier()
        if self.stop_after == ("P3", l):
            self.tap("YT", YT, [128, 12, S])
            return

        self.off = self.MBASE + 65536
        mergedT = self.A([128, KC, S], BF16)
        P5_OFF = self.off
        gbT = self.A([128, 24], F32)
        self.dma(sp, out=gbT, in_=self.gate_bT[l][:, :], writes=["gbT"], sem="small")
        wG = [self.A([128, KC, 3, 128], BF16) for _ in range(2)]
        wBr = [self.A([128, 4, 3, 128], BF16) for _ in range(2)]
        sgt = [self.A([128, 512], F32) for _ in range(2)]
        macc = self.A([128, 512], F32)
        mtmp = self.A([128, 512], F32)
        w_br = self.w_branch[l]
        assert self.off <= self.SLOT[1], ("P4 locals overlap wO slot", self.off, self.SLOT)

        def load_g(dc):
            s_ = dc % 2
            for n in range(3):
                c0 = 4104 + n * 1024 + dc * 128
                self.dma(pool, out=wG[s_][:, :, n, :], in_=w_in[:, :, c0:c0 + 128], writes=[f"wG{s_}"], sem=f"wG{s_}")
                self.dma(pool, out=wBr[s_][:, :, n, :],
                         in_=w_br[n].rearrange("(kc p) d -> p kc d", p=128)[:, :, dc * 128:(dc + 1) * 128],
                         writes=[f"wG{s_}"], sem=f"wG{s_}")
        load_g(0)
        cnt = 0
        for dc in range(KC):
            if dc + 1 < KC:
                load_g(dc + 1)
            s_ = dc % 2
            for g in range(4):
                gsl = slice(g * 512, (g + 1) * 512)
                for n in range(3):
                    pg, pb = cnt % 2, 2 + cnt % 2
                    sb_ = sgt[cnt % 2]
                    sk = f"sg{cnt % 2}"
                    cnt += 1
                    for kc in range(KC):
                        self.mm(B[pg], wG[s_][:, kc, n, :], xT[:, kc, gsl], kc == 0, kc == KC - 1, reads=[f"wG{s_}"], writes=[f"B{pg}"])
                    for kc in range(4):
                        self.mm(B[pb], wBr[s_][:, kc, n, :], YT[:, n * 4 + kc, gsl], kc == 0, kc == 3, reads=[f"wG{s_}"], writes=[f"B{pb}"])
                    self.actf(sb_, B[pg], AF.Sigmoid, reads=[f"B{pg}", "gbT"], writes=[sk],
                              bias=gbT[:, n * 8 + dc:n * 8 + dc + 1], scale=1.0)
                    if n == 0:
                        self.tt(macc, sb_, B[pb], ALU.mult, reads=[sk, f"B{pb}"], writes=["macc"])
                    elif n == 1:
                        self.tt(mtmp, sb_, B[pb], ALU.mult, reads=[sk, f"B{pb}"], writes=["mtmp"])
                        self.tt(macc, macc, mtmp, ALU.add, reads=["macc", "mtmp"], writes=["macc"])
                    else:
                        self.tt(mtmp, sb_, B[pb], ALU.mult, reads=[sk, f"B{pb}"], writes=["mtmp"])
                        self.tt(mergedT[:, dc, gsl], macc, mtmp, ALU.add, reads=["macc", "mtmp"], writes=[("mT", dc, g)])
        self.barrier()
        if self.stop_after == ("P4", l):
            self.tap("mergedT", mergedT, [128, KC, S])
            return

        self.off = self.MBASE
        acc = self.acc = self.A([128, NT, D], F32)
        self.off = P5_OFF
        moe = (l % 2 == 1)
        self.dma(sp, out=self.lng, in_=self.ln_gb[l][0:1, :].partition_broadcast(128), writes=["lng"], sem="lnp")
        self.dma(sp, out=self.lnb, in_=self.ln_gb[l][1:2, :].partition_broadcast(128), writes=["lnb"], sem="lnp")
        for r_ in ("lng", "lnb"):
            self.R(r_).w = (self.dsem("lnp"), self.dsem("lnp").cnt)
        xr = [self.A([128, D], F32) for _ in range(2)]
        sress = [self.A([128, D], F32) for _ in range(2)]
        xbf = self.A([128, D], BF16)
        xsrc = self.x if l == 0 else self.xres
        if moe:
            wR = self.A([128, KC, NEXP], F32)
            self.dma(sp, out=wR, in_=self.router.rearrange("(kc p) e -> p kc e", p=128), writes=["wR"], sem="small")
            x1T = self.A([128, KC, 128], F32)
            lgt = self.A([128, 8], F32)
            mx8 = self.A([128, 8], F32)
            nm1 = self.A([128, 1], F32)
            msk = self.A([128, 8], F32)
            eg = self.A([128, 8], F32)
            ssum = self.A([128, 1], F32)

        xbfs = [xbf, self.A([128, D], BF16)]
        assert self.off <= self.SLOT[1], ("P5 locals overlap wO slot", self.off, self.SLOT)

        def p5gen(i):
            tsl = slice(i * 128, (i + 1) * 128)
            b = i % 2
            sres = sress[b]
            sk_, ks = f"sres{b}", f"_{b}"
            xb_ = xbfs[b]
            st_, mv_, rs_ = self.lnt[b]
            self.dma(sp, out=xr[b], in_=xsrc[tsl, :], reads=[("xres", i)], writes=[f"xr{b}"], sem=f"xr{b}")
            for hf in range(2):
                pb_ = 2 * b + hf
                for kc in range(KC):
                    self.mm(B[pb_], mergedT[:, kc, tsl], wO[:, kc, hf * 512:(hf + 1) * 512], kc == 0, kc == KC - 1,
                            reads=["slot1"], writes=[f"B{pb_}"])
            yield
            for hf in range(2):
                pb_ = 2 * b + hf
                self.stt(sres[:, hf * 512:(hf + 1) * 512], xr[b][:, hf * 512:(hf + 1) * 512], float(ALPHA), B[pb_],
                         ALU.mult, ALU.add, reads=[f"xr{b}", f"B{pb_}"], writes=[sk_])
                yield
            for c in range(2):
                self.op(dve, lambda c=c: nc.vector.bn_stats(out=st_[:, c, :], in_=sres[:, c * 512:(c + 1) * 512]),
                        reads=[sk_], writes=["ln_st" + ks])
            yield
            self.op(dve, lambda: nc.vector.bn_aggr(out=mv_, in_=st_.rearrange("p a b -> p (a b)")), reads=["ln_st" + ks], writes=["ln_mv" + ks])
            yield
            self.actf(rs_, mv_[:, 1:2], AF.Sqrt, reads=["ln_mv" + ks], writes=["ln_rstd" + ks], bias=LN_EPS, scale=1.0)
            yield
            self.op(dve, lambda: nc.vector.reciprocal(out=rs_, in_=rs_), reads=["ln_rstd" + ks], writes=["ln_rstd" + ks])
            yield
            self.ts(sres, sres, mv_[:, 0:1], rs_[:, 0:1], ALU.subtract, ALU.mult, reads=[sk_, "ln_mv" + ks, "ln_rstd" + ks], writes=[sk_])
            yield
            self.tt(sres, sres, self.lng, ALU.mult, reads=[sk_, "lng"], writes=[sk_])
            yield
            self.tt(sres, sres, self.lnb, ALU.add, reads=[sk_, "lnb"], writes=[sk_])
            yield
            self.actf(xb_, sres, AF.Copy, reads=[sk_], writes=[f"xbf{b}"])
            self.actf(acc[:, i, :], sres, AF.Identity, reads=[sk_], writes=[("acc", i)], scale=float(ALPHA))
            yield
            psT = Bb[4 + b].rearrange("p (a b) -> p a b", a=8)
            for kc in range(KC):
                self.tr(psT[:, kc, :], xb_[:, kc * 128:(kc + 1) * 128], self.identb, reads=[f"xbf{b}"], writes=[f"B{4 + b}"], inc=(kc == KC - 1))
            yield
            self.actf(xT[:, :, tsl], psT, AF.Copy, reads=[f"B{4 + b}"], writes=[("xT", i)])
            yield
            if moe:
                for kc in range(KC):
                    bk = 6 + kc // 4
                    self.tr(B[bk][:, (kc % 4) * 128:(kc % 4 + 1) * 128], sres[:, kc * 128:(kc + 1) * 128], self.identf,
                            reads=[sk_, "cf"], writes=[f"B{bk}"], inc=(kc % 4 == 3))
                yield
                for hb in range(2):
                    self.vcopy(x1T[:, hb * 4:(hb + 1) * 4, :].rearrange("p a b -> p (a b)"), B[6 + hb],
                               reads=[f"B{6 + hb}"], writes=["x1T"])
                yield
                for kc in range(KC):
                    self.mm(B[6][:, 0:8], x1T[:, kc, :], wR[:, kc, :], kc == 0, kc == KC - 1, reads=["x1T", "wR"], writes=["B6"])
                yield
                self.vcopy(lgt, B[6][:, 0:8], reads=["B6"], writes=["lgt"])
                self.op(dve, lambda: nc.vector.max(out=mx8, in_=lgt), reads=["lgt"], writes=["mx8"])
                self.ts(nm1, mx8[:, 0:1], -1.0, None, ALU.mult, reads=["mx8"], writes=["nm1"])
                self.ts(msk, lgt, mx8[:, 1:2], None, ALU.is_ge, reads=["lgt", "mx8"], writes=["msk"])
                self.actf(eg, lgt, AF.Exp, reads=["lgt", "nm1"], writes=["eg"], bias=nm1[:, 0:1], scale=1.0)
                self.tt(eg, eg, msk, ALU.mult, reads=["eg", "msk"], writes=["eg"])
                self.op(dve, lambda: nc.vector.reduce_sum(out=ssum, in_=eg, axis=mybir.AxisListType.X), reads=["eg"], writes=["ssum"])
                self.op(dve, lambda: nc.vector.reciprocal(out=ssum, in_=ssum), reads=["ssum"], writes=["ssum"])
                self.ts(self.gates[:, i, :], eg, ssum[:, 0:1], None, ALU.mult, reads=["eg", "ssum"], writes=[("gates", i)])

        self.interleave(p5gen, NT, width=2, lag=5)
        self.barrier()
        if self.stop_after == ("P5", l):
            self.tap("acc", acc, [128, NT, D])
            self.tap("gates", self.gates, [128, NT, NEXP])
            return

    def ffn(self, l):
        nc = self.nc
        pe, act, dve, pool, sp = self.pe, self.act, self.dve, self.pool, self.sp
        xT = self.xT
        moe = (l % 2 == 1)
        last = (l == self.n_layers - 1)
        B = [self.bank(b) for b in range(8)]
        Bb = [self.bank(b, BF16) for b in range(8)]
        self.off = self.MBASE
        acc = self.A([128, NT, D], F32)
        hT = self.A([128, 4, S], BF16)
        upb = [self.A([128, KC, 2, 512], BF16) for _ in range(2)]
        dnb = [self.A([128, 4, D], BF16) for _ in range(2)]
        sa = [self.A([128, 512], F32) for _ in range(2)]
        if moe:
            F = FF_EXP
            segs = [(e, f0, 4) for e in range(getattr(self, 'dbg_nexp', NEXP)) for f0 in range(0, F // 128, 4)]
        else:
            F = FF_DENSE
            nch = F // 128
            segs = [(0, f0, min(4, nch - f0)) for f0 in range(0, nch, 4)]

        def wup(e):
            t = self.moe_up[e] if moe else self.dense_up
            return t.rearrange("(kc p) n -> p kc n", p=128)

        def wdn(e):
            t = self.moe_down[e] if moe else self.dense_down
            return t.rearrange("(fc p) d -> p fc d", p=128)

        def load(q):
            e, f0, nch = segs[q]
            s_ = q % 2
            w = nch * 128
            self.dma(pool, out=upb[s_][:, :, 0, 0:w], in_=wup(e)[:, :, f0 * 128:f0 * 128 + w], writes=[f"up{s_}"], sem=f"up{s_}")
            self.dma(pool, out=upb[s_][:, :, 1, 0:w], in_=wup(e)[:, :, F + f0 * 128:F + f0 * 128 + w], writes=[f"up{s_}"], sem=f"up{s_}")
            self.dma(pool, out=dnb[s_][:, 0:nch, :], in_=wdn(e)[:, f0:f0 + nch, :], writes=[f"dn{s_}"], sem=f"dn{s_}")

        load(0)
        ucnt = 0
        ocnt = 0
        for q, (e, f0, nch) in enumerate(segs):
            if q + 1 < len(segs):
                load(q + 1)
            s_ = q % 2
            for fc in range(nch):
                for g in range(4):
                    gsl = slice(g * 512, (g + 1) * 512)
                    pa, pb = ucnt % 2, 2 + ucnt % 2
                    sab = sa[ucnt % 2]
                    sk = f"sa{ucnt % 2}"
                    ucnt += 1
                    for kc in range(KC):
                        self.mm(B[pa], upb[s_][:, kc, 0, fc * 128:(fc + 1) * 128], xT[:, kc, gsl], kc == 0, kc == KC - 1,
                                reads=[f"up{s_}"], writes=[f"B{pa}"])
                    for kc in range(KC):
                        self.mm(B[pb], upb[s_][:, kc, 1, fc * 128:(fc + 1) * 128], xT[:, kc, gsl], kc == 0, kc == KC - 1,
                                reads=[f"up{s_}"], writes=[f"B{pb}"])
                    self.actf(sab, B[pa], AF.Silu, reads=[f"B{pa}"], writes=[sk])
                    self.tt(hT[:, fc, gsl], sab, B[pb], ALU.mult, reads=[sk, f"B{pb}"], writes=[("hT", fc, g)])
            for i in range(NT):
                tsl = slice(i * 128, (i + 1) * 128)
                for hf in range(2):
                    po = 4 + ocnt % 4
                    ocnt += 1
                    for fc in range(nch):
                        self.mm(B[po], hT[:, fc, tsl], dnb[s_][:, fc, hf * 512:(hf + 1) * 512], fc == 0, fc == nch - 1,
                                reads=[("hT", fc, i // 4), f"dn{s_}"], writes=[f"B{po}"])
                    asl = acc[:, i, hf * 512:(hf + 1) * 512]
                    if moe:
                        self.stt(asl, B[po], self.gates[:, i, e:e + 1], asl, ALU.mult, ALU.add,
                                 reads=[f"B{po}", ("acc", i, hf)], writes=[("acc", i, hf)])
                    else:
                        self.tt(asl, B[po], asl, ALU.add, reads=[f"B{po}", ("acc", i, hf)], writes=[("acc", i, hf)])
        self.barrier()
        if self.stop_after == ("F1", l):
            self.tap("acc", acc, [128, NT, D])
            return
        self.off = self.MBASE + 65536
        self.dma(sp, out=self.lng, in_=self.ln_gb[l][2:3, :].partition_broadcast(128), writes=["lng"], sem="lnp")
        self.dma(sp, out=self.lnb, in_=self.ln_gb[l][3:4, :].partition_broadcast(128), writes=["lnb"], sem="lnp")
        for r_ in ("lng", "lnb"):
            self.R(r_).w = (self.dsem("lnp"), self.dsem("lnp").cnt)
        xo = [self.A([128, D], F32) for _ in range(2)]
        xbfs = [self.A([128, D], BF16) for _ in range(2)]
        dst = self.y if last else self.xres
        if not last:
            self.prefetch_AB(l + 1)

        def lngen(i):
            tsl = slice(i * 128, (i + 1) * 128)
            b = i % 2
            ks = f"_{b}"
            st_, mv_, rs_ = self.lnt[b]
            ak = [("acc", i, 0), ("acc", i, 1)]
            for c in range(2):
                self.op(dve, lambda c=c: nc.vector.bn_stats(out=st_[:, c, :], in_=acc[:, i, c * 512:(c + 1) * 512]),
                        reads=ak, writes=["ln_st" + ks])
            yield
            self.op(dve, lambda: nc.vector.bn_aggr(out=mv_, in_=st_.rearrange("p a b -> p (a b)")), reads=["ln_st" + ks], writes=["ln_mv" + ks])
            yield
            self.actf(rs_, mv_[:, 1:2], AF.Sqrt, reads=["ln_mv" + ks], writes=["ln_rstd" + ks], bias=LN_EPS, scale=1.0)
            yield
            self.op(dve, lambda: nc.vector.reciprocal(out=rs_, in_=rs_), reads=["ln_rstd" + ks], writes=["ln_rstd" + ks])
            yield
            self.ts(xo[b], acc[:, i, :], mv_[:, 0:1], rs_[:, 0:1], ALU.subtract, ALU.mult,
                    reads=ak + ["ln_mv" + ks, "ln_rstd" + ks], writes=[f"xo{b}"])
            yield
            self.tt(xo[b], xo[b], self.lng, ALU.mult, reads=[f"xo{b}", "lng"], writes=[f"xo{b}"])
            yield
            self.tt(xo[b], xo[b], self.lnb, ALU.add, reads=[f"xo{b}", "lnb"], writes=[f"xo{b}"])
            yield
            self.dma(sp, out=dst[tsl, :], in_=xo[b], reads=[f"xo{b}"], writes=[("xres", i)], sem=f"xo{b}")
            if not last:
                self.actf(xbfs[b], xo[b], AF.Copy, reads=[f"xo{b}"], writes=[f"xbf{b}"])
                yield
                psT = Bb[b].rearrange("p (a b) -> p a b", a=8)
                for kc in range(KC):
                    self.tr(psT[:, kc, :], xbfs[b][:, kc * 128:(kc + 1) * 128], self.identb, reads=[f"xbf{b}"], writes=[f"B{b}"],
                            inc=(kc == KC - 1))
                yield
                self.actf(xT[:, :, tsl], psT, AF.Copy, reads=[f"B{b}"], writes=[("xT", i)])

        self.interleave(lngen, NT, width=2, lag=4)
        self.barrier()


def prep_inputs(inputs, n_layers=2):
    f = lambda a: np.ascontiguousarray(np.asarray(a, dtype=np.float32))
    cf, cb = host_consts()
    shared = {"constf": cf, "constb": cb}
    for l in range(n_layers):
        shared[f"w_in{l}"] = f(inputs["w_in"][l])
        shared[f"fox_b{l}"] = f(np.asarray(inputs["fox_b_f"])[l].reshape(1, 8))
        shared[f"gate_bT{l}"] = f(np.asarray(inputs["gate_b"])[l].reshape(24, 128).T)
        shared[f"gm_wsT{l}"] = f(np.asarray(inputs["gm_w_s"])[l].transpose(2, 0, 1).reshape(128, 512))
        shared[f"gm_bsT{l}"] = f(np.asarray(inputs["gm_b_s"])[l].T)
        shared[f"gm_ln{l}"] = f(np.stack([np.asarray(inputs["gm_ln_g"])[l], np.asarray(inputs["gm_ln_b"])[l]]))
        shared[f"w_branch{l}"] = f(inputs["w_branch"][l])
        shared[f"w_out{l}"] = f(inputs["w_out"][l])
        lg, lb = np.asarray(inputs["ln_g"])[l], np.asarray(inputs["ln_b"])[l]
        shared[f"ln_gb{l}"] = f(np.stack([lg[0], lb[0], lg[1], lb[1]]))
    shared["dense_up"] = f(inputs["dense_w_up"][0])
    shared["dense_down"] = f(inputs["dense_w_down"][0])
    if n_layers > 1:
        shared["router"] = f(inputs["moe_router"][0])
        shared["moe_up"] = f(inputs["moe_w_up"][0])
        shared["moe_down"] = f(inputs["moe_w_down"][0])
    return shared


def kernel(**inputs):
    x = np.asarray(inputs["x"], dtype=np.float32)
    nb = x.shape[0]
    kb = KB(n_layers=DEPTH)
    nc = kb.build()
    shared = prep_inputs(inputs, DEPTH)
    in_maps = []
    for b in range(nb):
        m = dict(shared)
        m["x"] = np.ascontiguousarray(x[b])
        in_maps.append(m)
    res = run_bass_kernel_spmd(nc, in_maps, core_ids=list(range(nb)))
    out = np.stack([np.asarray(res.results[b]["y"], dtype=np.float32) for b in range(nb)], axis=0)
    return out
```
